# Optimizing a Trainium2 kernel written in Bass

```python
import math
import jax
import jax.numpy as jnp
from jax import lax
import numpy as np

D_MODEL = 1024
BATCH = 32
SEQ = 2048
DEPTH = 4

CTX_LEN = 256
GRID_W = 64
ROPE_BASE = 10000.0
NORM_EPS = 1e-6
NEG_INF = -1e30

MLA_HEADS = 4
MLA_Q_RANK = 192
MLA_KV_RANK = 128
MLA_NOPE = 64
MLA_ROPE = 32
MLA_V = 64
Q_BLOCK = 128
NA_HEADS = 4
NA_DIM = 64
NA_ROWS = 8
NA_COLS = 16
NA_QCB = 16
NA_KBW = 32
S5_GROUPS = 16
S5_GROUP_CH = 16
S5_STATE = 64
S5_WIDTH = S5_GROUPS * S5_GROUP_CH
RET_HEADS = 4
RET_DIM = 64
RET_CHUNK = 128
MLA_WIDTH = MLA_HEADS * MLA_V
NA_WIDTH = NA_HEADS * NA_DIM
RET_WIDTH = RET_HEADS * RET_DIM
MIX_WIDTH = MLA_WIDTH + NA_WIDTH + S5_WIDTH + RET_WIDTH
IN_SPLITS = (MLA_Q_RANK, MLA_KV_RANK, MLA_ROPE, NA_WIDTH, NA_WIDTH, NA_WIDTH, S5_WIDTH,
             RET_WIDTH, RET_WIDTH, RET_WIDTH, RET_WIDTH)
IN_COLS = sum(IN_SPLITS)
MOE_GROUPS = 4
MOE_EXPERTS_PER_GROUP = 8
MOE_EXPERTS = MOE_GROUPS * MOE_EXPERTS_PER_GROUP
MOE_TOP_K = 2
MOE_FF = 512
MOE_BLOCK = 128

F32 = jnp.float32

kernel_name = 'hybrid_parallel_heads_diffusion_trunk'


def rmsnorm(x, g):
    xf = x.astype(F32)
    y = xf * lax.rsqrt(jnp.mean(xf * xf, axis=-1, keepdims=True) + NORM_EPS)
    return y.astype(x.dtype) * g


def split_cols(p):
    return jnp.split(p, list(np.cumsum(IN_SPLITS)[:-1]), axis=-1)


def axial_rope_tables(seq_len, rot_dim):
    t = jnp.arange(seq_len)
    row = (t // GRID_W).astype(F32)
    col = (t % GRID_W).astype(F32)
    half = rot_dim // 2
    inv = 1.0 / (ROPE_BASE ** (jnp.arange(0, half, 2, dtype=F32) / half))
    ar = row[:, None] * inv
    ac = col[:, None] * inv
    ang = jnp.concatenate([ar, ar, ac, ac], axis=-1)
    return jnp.cos(ang), jnp.sin(ang)


def _rotate_half(u):
    u1, u2 = jnp.split(u, 2, axis=-1)
    return jnp.concatenate([-u2, u1], axis=-1)


def apply_axial_rope(x, cos, sin):
    half = x.shape[-1] // 2
    rot = jnp.concatenate([_rotate_half(x[..., :half]), _rotate_half(x[..., half:])], axis=-1)
    return x * cos[None, :, None, :] + rot * sin[None, :, None, :]


def softmax_attention(q, k, v):
    s = jnp.einsum('bqhd,bkhd->bhqk', q, k, preferred_element_type=F32)
    p = jax.nn.softmax(s, axis=-1).astype(v.dtype)
    return jnp.einsum('bhqk,bkhe->bqhe', p, v)


def blocked_attention(q, k, v):
    B, L, H, dk = q.shape
    nb = L // Q_BLOCK
    qb = jnp.moveaxis(q.reshape(B, nb, Q_BLOCK, H, dk), 1, 0)
    out = lax.map(lambda qq: softmax_attention(qq, k, v), qb)
    return jnp.moveaxis(out, 0, 1).reshape(B, L, H, v.shape[-1])


def mla_queries(cq, g_cq, w_uq, cos, sin):
    B, L, _ = cq.shape
    q = (rmsnorm(cq, g_cq) @ w_uq).reshape(B, L, MLA_HEADS, MLA_NOPE + MLA_ROPE)
    q_nope, q_rope = q[..., :MLA_NOPE], q[..., MLA_NOPE:]
    if cos is not None:
        q_rope = apply_axial_rope(q_rope, cos, sin)
    return jnp.concatenate([q_nope, q_rope], axis=-1) * (MLA_NOPE + MLA_ROPE) ** -0.5


def mla_keys_values(ckv, kr, g_ckv, w_ukv, cos, sin):
    B, L, _ = ckv.shape
    kv = (rmsnorm(ckv, g_ckv) @ w_ukv).reshape(B, L, MLA_HEADS, MLA_NOPE + MLA_V)
    k_nope, v = kv[..., :MLA_NOPE], kv[..., MLA_NOPE:]
    k_rope = kr[:, :, None, :]
    if cos is not None:
        k_rope = apply_axial_rope(k_rope, cos, sin)
    k = jnp.concatenate([k_nope, jnp.broadcast_to(k_rope, (B, L, MLA_HEADS, MLA_ROPE))], axis=-1)
    return k, v


def mla_mixer(cq_l, ckv_l, kr_l, cq_c, ckv_c, kr_c, g_cq, g_ckv, w_uq, w_ukv, cos, sin, need_ctx):
    B, L, _ = cq_l.shape
    q_l = mla_queries(cq_l, g_cq, w_uq, cos, sin)
    k_l, v_l = mla_keys_values(ckv_l, kr_l, g_ckv, w_ukv, cos, sin)
    k_c, v_c = mla_keys_values(ckv_c, kr_c, g_ckv, w_ukv, None, None)
    out_l = blocked_attention(q_l, jnp.concatenate([k_l, k_c], axis=1),
                              jnp.concatenate([v_l, v_c], axis=1)).reshape(B, L, MLA_WIDTH)
    out_c = None
    if need_ctx:
        q_c = mla_queries(cq_c, g_cq, w_uq, None, None)
        out_c = softmax_attention(q_c, k_c, v_c).reshape(B, cq_c.shape[1], MLA_WIDTH)
    return out_l, out_c


def natten_mixer(q_l, k_l, v_l, q_c, k_c, v_c, rpb, need_ctx):
    B, L, _ = q_l.shape
    Lc = k_c.shape[1]
    rows = L // GRID_W
    wr = min(NA_ROWS, rows)
    scale = NA_DIM ** -0.5

    def heads(t):
        return t.reshape(t.shape[0], t.shape[1], NA_HEADS, NA_DIM)

    qg = (heads(q_l) * scale).reshape(B, rows, GRID_W, NA_HEADS, NA_DIM)
    kg = heads(k_l).reshape(B, rows, GRID_W, NA_HEADS, NA_DIM)
    vg = heads(v_l).reshape(B, rows, GRID_W, NA_HEADS, NA_DIM)
    kc_h, vc_h = heads(k_c), heads(v_c)
    n_cb = GRID_W // NA_QCB
    q_cols = np.arange(GRID_W).reshape(n_cb, NA_QCB)
    q_start = np.clip(q_cols - NA_COLS // 2, 0, GRID_W - NA_COLS)
    blk_start = np.clip(np.arange(n_cb) * NA_QCB - NA_COLS // 2, 0, GRID_W - NA_KBW)
    key_cols = blk_start[:, None] + np.arange(NA_KBW)
    kcol = key_cols[:, None, :]
    col_valid = (kcol >= q_start[..., None]) & (kcol < q_start[..., None] + NA_COLS)
    dc_idx = np.clip(kcol - q_cols[..., None] + NA_COLS - 1, 0, 2 * NA_COLS - 2)
    bias_cols = jnp.transpose(rpb[:, :, dc_idx], (0, 2, 3, 1, 4))
    valid = col_valid[:, :, None, :]
    n_win = wr * NA_KBW

    def one_row(r):
        rs = jnp.clip(r - wr // 2, 0, rows - wr)
        k_blk = lax.dynamic_slice_in_dim(kg, rs, wr, axis=1)[:, :, key_cols]
        v_blk = lax.dynamic_slice_in_dim(vg, rs, wr, axis=1)[:, :, key_cols]
        q_row = lax.dynamic_index_in_dim(qg, r, axis=1, keepdims=False).reshape(B, n_cb, NA_QCB, NA_HEADS, NA_DIM)
        dr_idx = rs + jnp.arange(wr) - r + NA_ROWS - 1
        bias = jnp.take(bias_cols, dr_idx, axis=3)
        s_win = jnp.einsum('bjqhd,brjkhd->bhjqrk', q_row, k_blk, preferred_element_type=F32) + bias
        s_win = jnp.where(valid, s_win, NEG_INF).reshape(B, NA_HEADS, n_cb, NA_QCB, n_win)
        s_ctx = jnp.einsum('bjqhd,bkhd->bhjqk', q_row, kc_h, preferred_element_type=F32)
        p = jax.nn.softmax(jnp.concatenate([s_win, s_ctx], axis=-1), axis=-1).astype(v_blk.dtype)
        p_win = p[..., :n_win].reshape(B, NA_HEADS, n_cb, NA_QCB, wr, NA_KBW)
        o = (jnp.einsum('bhjqrk,brjkhd->bjqhd', p_win, v_blk)
             + jnp.einsum('bhjqk,bkhd->bjqhd', p[..., n_win:], vc_h))
        return o.reshape(B, GRID_W, NA_WIDTH)

    out_l = jnp.moveaxis(lax.map(one_row, jnp.arange(rows)), 0, 1).reshape(B, L, NA_WIDTH)
    out_c = None
    if need_ctx:
        out_c = softmax_attention(heads(q_c) * scale, kc_h, vc_h).reshape(B, Lc, NA_WIDTH)
    return out_l, out_c


def s5_discretise(a_re, a_im, log_dt, b_re, b_im):
    a_re, a_im = a_re.astype(F32), a_im.astype(F32)
    dt = jnp.exp(log_dt.astype(F32))[:, None]
    ldr, ldi = a_re * dt, a_im * dt
    mag = jnp.exp(ldr)
    abar_re, abar_im = mag * jnp.cos(ldi), mag * jnp.sin(ldi)
    num_re, num_im = abar_re - 1.0, abar_im
    den = a_re * a_re + a_im * a_im
    f_re = (num_re * a_re + num_im * a_im) / den
    f_im = (num_im * a_re - num_re * a_im) / den
    b_re, b_im = b_re.astype(F32), b_im.astype(F32)
    bb_re = f_re[..., None] * b_re - f_im[..., None] * b_im
    bb_im = f_re[..., None] * b_im + f_im[..., None] * b_re
    return abar_re, abar_im, bb_re, bb_im, ldr, ldi


def _complex_affine_combine(left, right):
    a1r, a1i, b1r, b1i = left
    a2r, a2i, b2r, b2i = right
    return (a2r * a1r - a2i * a1i, a2r * a1i + a2i * a1r,
            a2r * b1r - a2i * b1i + b2r, a2r * b1i + a2i * b1r + b2i)


def s5_scan(bu_re, bu_im, abar_re, abar_im, reverse):
    L = bu_re.shape[1]
    a_re = jnp.broadcast_to(abar_re, (1, L) + abar_re.shape)
    a_im = jnp.broadcast_to(abar_im, (1, L) + abar_im.shape)
    return lax.associative_scan(_complex_affine_combine, (a_re, a_im, bu_re, bu_im), reverse=reverse, axis=1)


def s5_readout(h_re, h_im, c_re, c_im):
    return jnp.einsum('blgp,gcp->blgc', h_re, c_re) - jnp.einsum('blgp,gcp->blgc', h_im, c_im)


def s5_mixer(u_l, u_c, a_re, a_im, log_dt, b_re, b_im, c_re, c_im, d_skip, w_glu, b_glu, need_ctx):
    B, L, _ = u_l.shape
    Lc = u_c.shape[1]
    ul_g = u_l.astype(F32).reshape(B, L, S5_GROUPS, S5_GROUP_CH)
    uc_g = u_c.astype(F32).reshape(B, Lc, S5_GROUPS, S5_GROUP_CH)
    y_l, y_c = [], []
    for d in range(2):
        abr, abi, bbr, bbi, ldr, ldi = s5_discretise(a_re[d], a_im[d], log_dt[d], b_re[d], b_im[d])
        cr, ci = c_re[d].astype(F32), c_im[d].astype(F32)
        buc_re = jnp.einsum('blgc,gpc->blgp', uc_g, bbr)
        buc_im = jnp.einsum('blgc,gpc->blgp', uc_g, bbi)
        dist = jnp.arange(Lc, dtype=F32)
        if d == 0:
            dist = (Lc - 1) - dist
        pw_mag = jnp.exp(dist[:, None, None] * ldr)
        pw_re = pw_mag * jnp.cos(dist[:, None, None] * ldi)
        pw_im = pw_mag * jnp.sin(dist[:, None, None] * ldi)
        h0_re = jnp.einsum('lgp,blgp->bgp', pw_re, buc_re) - jnp.einsum('lgp,blgp->bgp', pw_im, buc_im)
        h0_im = jnp.einsum('lgp,blgp->bgp', pw_re, buc_im) + jnp.einsum('lgp,blgp->bgp', pw_im, buc_re)
        bul_re = jnp.einsum('blgc,gpc->blgp', ul_g, bbr)
        bul_im = jnp.einsum('blgc,gpc->blgp', ul_g, bbi)
        pa_re, pa_im, h_re, h_im = s5_scan(bul_re, bul_im, abr, abi, reverse=(d == 1))
        h_re = h_re + pa_re * h0_re[:, None] - pa_im * h0_im[:, None]
        h_im = h_im + pa_re * h0_im[:, None] + pa_im * h0_re[:, None]
        y_l.append(s5_readout(h_re, h_im, cr, ci))
        if need_ctx:
            _, _, hc_re, hc_im = s5_scan(buc_re, buc_im, abr, abi, reverse=(d == 1))
            y_c.append(s5_readout(hc_re, hc_im, cr, ci))

    def finish(ys, u):
        y = (ys[0] + ys[1]).reshape(u.shape) + d_skip * u.astype(F32)
        g = jax.nn.gelu(y)
        return (g * jax.nn.sigmoid(g @ w_glu + b_glu)).astype(u.dtype)

    out_l = finish(y_l, u_l)
    out_c = finish(y_c, u_c) if need_ctx else None
    return out_l, out_c


def retention_chunkwise(q, k, v, lg, r0, strict):
    B, L, H, d = q.shape
    n = L // RET_CHUNK

    def chunks(t):
        return jnp.moveaxis(t.reshape(B, n, RET_CHUNK, H, t.shape[-1]), 1, 0)

    pos = jnp.arange(RET_CHUNK, dtype=F32)
    diff = pos[:, None] - pos[None, :]
    mask = diff > 0 if strict else diff >= 0
    inner_decay = jnp.where(mask[None], jnp.exp(lg[:, None, None] * jnp.maximum(diff, 0.0)[None]), 0.0)
    q_decay = jnp.exp(lg[None, :] * (pos[:, None] + 1.0))[None, :, :, None]
    k_decay = jnp.exp(lg[None, :] * (RET_CHUNK - 1.0 - pos)[:, None])[None, :, :, None]
    chunk_decay = jnp.exp(lg * RET_CHUNK)[None, :, None, None]

    def step(state, inp):
        qc, kc, vc = inp
        s = jnp.einsum('bqhd,bkhd->bhqk', qc, kc) * inner_decay
        inner = jnp.einsum('bhqk,bkhe->bqhe', s, vc)
        cross = jnp.einsum('bqhd,bhde->bqhe', qc, state) * q_decay
        state = state * chunk_decay + jnp.einsum('bkhd,bkhe->bhde', kc * k_decay, vc)
        return state, inner + cross

    _, out = lax.scan(step, r0, (chunks(q), chunks(k), chunks(v)))
    return jnp.moveaxis(out, 0, 1).reshape(B, L, H, v.shape[-1])


def retention_direction(q, k, v, lg, r0, reverse):
    if reverse:
        q, k, v = jnp.flip(q, 1), jnp.flip(k, 1), jnp.flip(v, 1)
        return jnp.flip(retention_chunkwise(q, k, v, lg, r0, strict=True), 1)
    return retention_chunkwise(q, k, v, lg, r0, strict=False)


def retention_context_state(k, v, lg, reverse):
    Lc = k.shape[1]
    dist = jnp.arange(Lc, dtype=F32)
    if not reverse:
        dist = (Lc - 1) - dist
    w = jnp.exp(dist[:, None] * lg[None, :])
    return jnp.einsum('blhd,blhe,lh->bhde', k, v, w)


def head_group_norm(y):
    mu = jnp.mean(y, axis=-1, keepdims=True)
    var = jnp.mean(jnp.square(y - mu), axis=-1, keepdims=True)
    return (y - mu) * lax.rsqrt(var + NORM_EPS)


def retention_mixer(q_l, k_l, v_l, g_l, q_c, k_c, v_c, g_c, log_decay, cos, sin, need_ctx):
    B, L, _ = q_l.shape
    Lc = k_c.shape[1]

    def heads(t):
        return t.astype(F32).reshape(t.shape[0], t.shape[1], RET_HEADS, RET_DIM)

    kscale = RET_DIM ** -0.5
    ql_h = apply_axial_rope(heads(q_l), cos, sin)
    kl_h = apply_axial_rope(heads(k_l), cos, sin) * kscale
    vl_h = heads(v_l)
    kc_h, vc_h = heads(k_c) * kscale, heads(v_c)
    y_l, y_c = [], []
    for d in range(2):
        lg = log_decay[d].astype(F32)
        r0 = retention_context_state(kc_h, vc_h, lg, reverse=(d == 1))
        y_l.append(retention_direction(ql_h, kl_h, vl_h, lg, r0, reverse=(d == 1)))
        if need_ctx:
            zero = jnp.zeros((B, RET_HEADS, RET_DIM, RET_DIM), F32)
            y_c.append(retention_direction(heads(q_c), kc_h, vc_h, lg, zero, reverse=(d == 1)))
    out_l = (jax.nn.silu(g_l.astype(F32)) * head_group_norm(y_l[0] + y_l[1]).reshape(B, L, RET_WIDTH)).astype(q_l.dtype)
    out_c = None
    if need_ctx:
        out_c = (jax.nn.silu(g_c.astype(F32)) * head_group_norm(y_c[0] + y_c[1]).reshape(B, Lc, RET_WIDTH)).astype(q_c.dtype)
    return out_l, out_c


def moe_ffn(h, w_group, b_group, w_expert, b_expert, w1, w3, w2):
    T, D = h.shape
    g_logits = jnp.matmul(h, w_group, preferred_element_type=F32) + b_group
    g_prob = jax.nn.softmax(g_logits, axis=-1)
    g_idx = jnp.argmax(g_logits, axis=-1)
    g_gate = jnp.take_along_axis(g_prob, g_idx[:, None], axis=-1)
    e_logits = (jnp.matmul(h, w_expert, preferred_element_type=F32) + b_expert).reshape(T, MOE_GROUPS, MOE_EXPERTS_PER_GROUP)
    e_logits = jnp.take_along_axis(e_logits, g_idx[:, None, None], axis=1)[:, 0]
    top_p, top_i = lax.top_k(jax.nn.softmax(e_logits, axis=-1), MOE_TOP_K)
    gates = g_gate * top_p / jnp.sum(top_p, axis=-1, keepdims=True)
    expert = g_idx[:, None] * MOE_EXPERTS_PER_GROUP + top_i
    n_pairs = T * MOE_TOP_K
    flat_e = expert.reshape(n_pairs).astype(jnp.int32)
    flat_tok = jnp.repeat(jnp.arange(T, dtype=jnp.int32), MOE_TOP_K)
    flat_w = gates.reshape(n_pairs)
    order = jnp.argsort(flat_e)
    sorted_e = flat_e[order]
    counts = jnp.bincount(flat_e, length=MOE_EXPERTS)
    starts = jnp.cumsum(counts) - counts
    padded = (counts + MOE_BLOCK - 1) // MOE_BLOCK * MOE_BLOCK
    pad_ends = jnp.cumsum(padded)
    pad_starts = pad_ends - padded
    dest = pad_starts[sorted_e] + jnp.arange(n_pairs, dtype=jnp.int32) - starts[sorted_e]
    cap = -(-(n_pairs + MOE_EXPERTS * (MOE_BLOCK - 1)) // MOE_BLOCK) * MOE_BLOCK
    n_blocks = cap // MOE_BLOCK
    slot_tok = jnp.full((cap,), T, jnp.int32).at[dest].set(flat_tok[order])
    slot_w = jnp.zeros((cap,), F32).at[dest].set(flat_w[order])
    h_pad = jnp.concatenate([h, jnp.zeros((1, D), h.dtype)], axis=0)
    buf = h_pad[slot_tok].reshape(n_blocks, MOE_BLOCK, D)
    block_expert = jnp.minimum(jnp.searchsorted(pad_ends, jnp.arange(n_blocks, dtype=jnp.int32) * MOE_BLOCK, side='right'), MOE_EXPERTS - 1)

    def expert_block(args):
        xb, e = args
        return (jax.nn.silu(xb @ w1[e]) * (xb @ w3[e])) @ w2[e]

    y = lax.map(expert_block, (buf, block_expert)).reshape(cap, D)
    return jax.ops.segment_sum(y * slot_w[:, None].astype(y.dtype), slot_tok, num_segments=T + 1)[:T]


def setup_inputs(seed: int = 0) -> dict:
    key = jax.random.key(seed)
    ks = iter(jax.random.split(key, 48))

    def nrm(shape, scale):
        return jax.random.normal(next(ks), shape, F32) * scale

    L, D = DEPTH, D_MODEL
    G, P, CG = S5_GROUPS, S5_STATE, S5_GROUP_CH
    ret_base = jnp.log1p(-(2.0 ** (-5.0 - jnp.arange(RET_HEADS, dtype=F32))))
    return {
        'x': nrm((BATCH, SEQ, D), 1.0),
        'c': nrm((BATCH, D), 1.0),
        'ctx': nrm((BATCH, CTX_LEN, D), 1.0),
        'c_ctx': nrm((D,), 1.0),
        'w_mod': nrm((L, D, 6 * D), 0.5 * D ** -0.5),
        'b_mod': nrm((L, 6 * D), 0.02),
        'g_mix': 1.0 + nrm((L, D), 0.02),
        'g_ffn': 1.0 + nrm((L, D), 0.02),
        'w_in': nrm((L, D, IN_COLS), D ** -0.5),
        'mla_g_cq': 1.0 + nrm((L, MLA_Q_RANK), 0.02),
        'mla_g_ckv': 1.0 + nrm((L, MLA_KV_RANK), 0.02),
        'mla_w_uq': nrm((L, MLA_Q_RANK, MLA_HEADS * (MLA_NOPE + MLA_ROPE)), MLA_Q_RANK ** -0.5),
        'mla_w_ukv': nrm((L, MLA_KV_RANK, MLA_HEADS * (MLA_NOPE + MLA_V)), MLA_KV_RANK ** -0.5),
        'na_rpb': nrm((L, NA_HEADS, 2 * NA_ROWS - 1, 2 * NA_COLS - 1), 0.1),
        's5_a_re': -0.5 * (1.0 + nrm((L, 2, G, P), 0.05)),
        's5_a_im': jnp.pi * jnp.arange(P, dtype=F32) + nrm((L, 2, G, P), 0.05),
        's5_log_dt': jax.random.uniform(next(ks), (L, 2, G), F32, math.log(1e-3), math.log(1e-1)),
        's5_b_re': nrm((L, 2, G, P, CG), (2.0 * CG) ** -0.5),
        's5_b_im': nrm((L, 2, G, P, CG), (2.0 * CG) ** -0.5),
        's5_c_re': nrm((L, 2, G, CG, P), (2.0 * P) ** -0.5),
        's5_c_im': nrm((L, 2, G, CG, P), (2.0 * P) ** -0.5),
        's5_d': nrm((L, S5_WIDTH), 1.0),
        's5_w_glu': nrm((L, S5_WIDTH, S5_WIDTH), S5_WIDTH ** -0.5),
        's5_b_glu': nrm((L, S5_WIDTH), 0.02),
        'ret_log_decay': ret_base * (1.0 + nrm((L, 2, RET_HEADS), 0.05)),
        'w_out': nrm((L, MIX_WIDTH, D), MIX_WIDTH ** -0.5),
        'moe_w_group': nrm((L, D, MOE_GROUPS), D ** -0.5),
        'moe_b_group': nrm((L, MOE_GROUPS), 0.01),
        'moe_w_expert': nrm((L, D, MOE_EXPERTS), D ** -0.5),
        'moe_b_expert': nrm((L, MOE_EXPERTS), 0.01),
        'moe_w1': nrm((L, MOE_EXPERTS, D, MOE_FF), D ** -0.5),
        'moe_w3': nrm((L, MOE_EXPERTS, D, MOE_FF), D ** -0.5),
        'moe_w2': nrm((L, MOE_EXPERTS, MOE_FF, D), MOE_FF ** -0.5),
        'g_final': 1.0 + nrm((D,), 0.02),
    }


def reference(x, c, ctx, c_ctx, w_mod, b_mod, g_mix, g_ffn, w_in, mla_g_cq, mla_g_ckv, mla_w_uq, mla_w_ukv,
              na_rpb, s5_a_re, s5_a_im, s5_log_dt, s5_b_re, s5_b_im, s5_c_re, s5_c_im, s5_d, s5_w_glu, s5_b_glu,
              ret_log_decay, w_out, moe_w_group, moe_b_group, moe_w_expert, moe_b_expert, moe_w1, moe_w3, moe_w2,
              g_final):
    B, S, D = x.shape
    cos_m, sin_m = axial_rope_tables(S, MLA_ROPE)
    cos_r, sin_r = axial_rope_tables(S, RET_DIM)
    c_act = jax.nn.silu(c)
    cc_act = jax.nn.silu(c_ctx)
    xl, xc = x, ctx
    for layer in range(DEPTH):
        need_ctx = layer < DEPTH - 1
        sh1, sc1, gt1, sh2, sc2, gt2 = [m[:, None, :] for m in jnp.split(c_act @ w_mod[layer] + b_mod[layer], 6, axis=-1)]
        csh1, csc1, cgt1, csh2, csc2, cgt2 = jnp.split(cc_act @ w_mod[layer] + b_mod[layer], 6, axis=-1)
        hl = rmsnorm(xl, g_mix[layer]) * (1.0 + sc1) + sh1
        hc = rmsnorm(xc, g_mix[layer]) * (1.0 + csc1) + csh1
        pl = split_cols(hl @ w_in[layer])
        pc = split_cols(hc @ w_in[layer])
        a_l, a_c = mla_mixer(pl[0], pl[1], pl[2], pc[0], pc[1], pc[2], mla_g_cq[layer], mla_g_ckv[layer],
                             mla_w_uq[layer], mla_w_ukv[layer], cos_m, sin_m, need_ctx)
        n_l, n_c = natten_mixer(pl[3], pl[4], pl[5], pc[3], pc[4], pc[5], na_rpb[layer], need_ctx)
        s_l, s_c = s5_mixer(pl[6], pc[6], s5_a_re[layer], s5_a_im[layer], s5_log_dt[layer], s5_b_re[layer],
                            s5_b_im[layer], s5_c_re[layer], s5_c_im[layer], s5_d[layer], s5_w_glu[layer],
                            s5_b_glu[layer], need_ctx)
        r_l, r_c = retention_mixer(pl[7], pl[8], pl[9], pl[10], pc[7], pc[8], pc[9], pc[10], ret_log_decay[layer],
                                   cos_r, sin_r, need_ctx)
        xl = xl + gt1 * (jnp.concatenate([a_l, n_l, s_l, r_l], axis=-1) @ w_out[layer])
        if need_ctx:
            xc = xc + cgt1 * (jnp.concatenate([a_c, n_c, s_c, r_c], axis=-1) @ w_out[layer])
        fl = rmsnorm(xl, g_ffn[layer]) * (1.0 + sc2) + sh2
        moe_w = (moe_w_group[layer], moe_b_group[layer], moe_w_expert[layer], moe_b_expert[layer],
                 moe_w1[layer], moe_w3[layer], moe_w2[layer])
        if need_ctx:
            fc = rmsnorm(xc, g_ffn[layer]) * (1.0 + csc2) + csh2
            y = moe_ffn(jnp.concatenate([fl.reshape(-1, D), fc.reshape(-1, D)], axis=0), *moe_w)
            xl = xl + gt2 * y[:B * S].reshape(B, S, D)
            xc = xc + cgt2 * y[B * S:].reshape(xc.shape)
        else:
            xl = xl + gt2 * moe_ffn(fl.reshape(-1, D), *moe_w).reshape(B, S, D)
    return rmsnorm(xl, g_final)
```

```python
import contextlib
import numpy as np
import concourse.bass as bass
import concourse.mybir as mybir
from concourse.bass_utils import run_bass_kernel_spmd

F32 = mybir.dt.float32
BF16 = mybir.dt.bfloat16
I32 = mybir.dt.int32
AF = mybir.ActivationFunctionType
ALU = mybir.AluOpType
AX = mybir.AxisListType

D = 1024
TL = 2048
TC = 256
T = TL + TC
DEPTH = 4
NCORES = 8
EPS = 1e-6
NEG = -30000.0
R_CQ, R_CKV, R_KR, R_KRP, R_NAQ, R_NAK, R_S5, R_RQ, R_RK, R_RG, R_RQP, R_RKP = (
    0, 192, 320, 352, 384, 640, 896, 1152, 1408, 1664, 1920, 2176)
NPROJ = 2432
NE = 32
FF = 512


class Res:
    __slots__ = ("name", "w", "r")

    def __init__(self, name=""):
        self.name = name
        self.w = None
        self.r = {}


class Em:
    COMPUTE = ("pe", "act", "dve", "pool")
    ALL = ("pe", "act", "dve", "pool", "sp")
    RING = {"sp": 12, "pool": 8, "act": 4}

    def __init__(self, nc):
        self.nc = nc
        self.ops = {e: [] for e in self.ALL}
        self.cnt = {e: 0 for e in self.COMPUTE}
        self.seen = {e: {} for e in self.ALL}
        self.rings = {q: [["d_%s_%d" % (q, i), 0] for i in range(n)] for q, n in self.RING.items()}
        self.rpos = {q: 0 for q in self.RING}
        self.semkeys = ["c_" + e for e in self.COMPUTE] + [s[0] for q in self.rings for s in self.rings[q]]
        self.out_tokens = []

    def _need(self, eng, toks):
        best = {}
        for t in toks:
            if t is None:
                continue
            k, v = t
            if k == "c_" + eng:
                if eng == "pe" or self.cnt[eng] + 1 - v > 6:
                    continue
            if self.seen[eng].get(k, 0) >= v:
                continue
            if best.get(k, 0) < v:
                best[k] = v
        for k, v in best.items():
            self.seen[eng][k] = v
        return list(best.items())

    def _deps(self, eng, r, w):
        toks = []
        for x in r:
            toks.append(x.w)
        for x in w:
            toks.append(x.w)
            toks.extend(x.r.items())
        return self._need(eng, toks)

    def _mark(self, tok, r, w):
        for x in r:
            if x.r.get(tok[0], 0) < tok[1]:
                x.r[tok[0]] = tok[1]
        for x in w:
            x.w = tok
            x.r = {}

    def op(self, eng, fn, r=(), w=()):
        waits = self._deps(eng, r, w)
        self.cnt[eng] += 1
        tok = ("c_" + eng, self.cnt[eng])
        self.ops[eng].append((waits, fn, tok[0], 1))
        self._mark(tok, r, w)
        return tok

    def dma(self, q, fn, r=(), w=(), is_out=False):
        ring = self.rings[q]
        slot = ring[self.rpos[q] % len(ring)]
        self.rpos[q] += 1
        toks = [(slot[0], 16 * slot[1])] if slot[1] > 0 else []
        waits = self._need(q, toks) + self._deps(q, r, w)
        slot[1] += 1
        tok = (slot[0], 16 * slot[1])
        self.ops[q].append((waits, fn, slot[0], 16))
        self._mark(tok, r, w)
        if is_out:
            self.out_tokens.append(tok)
        return tok

    def barrier(self):
        toks = [("c_" + e, self.cnt[e]) for e in self.COMPUTE if self.cnt[e] > 0]
        for q in self.rings:
            for s in self.rings[q]:
                if s[1] > 0:
                    toks.append((s[0], 16 * s[1]))
        for e in self.ALL:
            waits = self._need(e, toks)
            if waits:
                self.ops[e].append((waits, None, None, 0))

    def finish(self):
        nc = self.nc
        self.barrier()
        with contextlib.ExitStack() as st:
            sems = {k: st.enter_context(nc.semaphore(k)) for k in self.semkeys}
            block = st.enter_context(nc.Block())

            def run(eng_name):
                def body(e):
                    for waits, fn, sk, inc in self.ops[eng_name]:
                        for k, v in waits:
                            e.wait_ge(sems[k], v)
                        if fn is not None:
                            ins = fn(e)
                            ins.then_inc(sems[sk], inc)
                return body

            block.tensor(run("pe"))
            block.scalar(run("act"))
            block.vector(run("dve"))
            block.gpsimd(run("pool"))
            block.sync(run("sp"))


class Arena:
    def __init__(self, ap, words):
        self.ap = ap
        self.words = words
        self.top = 0
        self.marks = []

    def alloc(self, shape, dt):
        n = 1
        for s in shape[1:]:
            n *= s
        w = (n + 1) // 2 if dt == BF16 else n
        w = (w + 1) // 2 * 2
        assert self.top + w <= self.words, ("arena overflow", self.top, w, self.words)
        v = self.ap[:, self.top:self.top + w]
        self.top += w
        if dt == BF16:
            v = v.bitcast(BF16)
        elif dt == I32:
            v = v.bitcast(I32)
        v = v[:, 0:n]
        if len(shape) == 3:
            v = v.rearrange("p (a b) -> p a b", a=shape[1])
        elif len(shape) == 4:
            v = v.rearrange("p (a b c) -> p a b c", a=shape[1], b=shape[2])
        if shape[0] < 128:
            v = v[0:shape[0]]
        return v

    def push(self):
        self.marks.append(self.top)

    def pop(self):
        self.top = self.marks.pop()


def chunks_of(total, step):
    return [(s, min(step, total - s)) for s in range(0, total, step)]


TCH = [(0, 512), (512, 512), (1024, 512), (1536, 512), (2048, 256)]


class Builder:
    def __init__(self, BL, NL, dbg=(), upto="all"):
        self.BL, self.NL, self.dbg, self.upto = BL, NL, set(dbg), upto
        self.NB1 = BL + 1
        nc = self.nc = bass.Bass("TRN2", target_bir_lowering=False)
        self.em = Em(nc)
        self.din = {}
        self.dsc = {}
        self.R = {}

    def inp(self, name, shape, dt=F32):
        self.din[name] = self.nc.dram_tensor(name, list(shape), dt, kind="ExternalInput").ap()
        self.R[name] = Res(name)
        return self.din[name]

    def scratch(self, name, shape, dt, out=False):
        kind = "ExternalOutput" if (out or name in self.dbg) else "Internal"
        self.dsc[name] = self.nc.dram_tensor(name, list(shape), dt, kind=kind).ap()
        self.R[name] = Res(name)
        return self.dsc[name]

    def res(self, name):
        if name not in self.R:
            self.R[name] = Res(name)
        return self.R[name]

    def build(self):
        nc, em, BL, NL, NB1 = self.nc, self.em, self.BL, self.NL, self.NB1
        L = DEPTH
        inp = self.inp
        xin = inp("xin", [BL, D, T])
        cc = inp("cc", [NB1, D])
        w_mod = inp("w_mod", [L, D, 6 * D])
        b_mod = inp("b_mod", [L, 6 * D])
        g_mix = inp("g_mix", [L, D])
        g_ffn = inp("g_ffn", [L, D])
        w_fm = inp("w_fm", [L, D, NPROJ])
        w_v = inp("w_v", [L, D, 512])
        g_final = inp("g_final", [D])
        inp("mla_g_cq", [L, 192]); inp("mla_g_ckv", [L, 128]); inp("mla_w_uq", [L, 192, 384])
        inp("w_uq_rot", [L, 192, 4, 32]); inp("mla_w_ukv", [L, 128, 512]); inp("ropem", [2, 32, T])
        inp("na_tc", [L, 4, 23, 64, 64]); inp("na_mask", [28, 128, 512])
        inp("ret_log_decay", [L, 2, 4]); inp("retd", [38, 2, 128, 512]); inp("roper", [2, 64, T])
        inp("s5_a_re", [L, 2, 16, 64]); inp("s5_a_im", [L, 2, 16, 64]); inp("s5_log_dt", [L, 2, 16])
        inp("s5_b_re", [L, 2, 16, 64, 16]); inp("s5_b_im", [L, 2, 16, 64, 16])
        inp("s5_c_re", [L, 2, 16, 16, 64]); inp("s5_c_im", [L, 2, 16, 16, 64])
        inp("s5_d", [L, 256]); inp("s5_w_glu", [L, 256, 256]); inp("s5_b_glu", [L, 256])
        inp("cst", [128, 272]); inp("cst2", [128, 322])
        inp("w_out", [L, D, D]); inp("moe_w_group", [L, D, 4]); inp("moe_b_group", [L, 4])
        inp("moe_w_expert", [L, D, 32]); inp("moe_b_expert", [L, 32])
        inp("moe_w1", [L, NE, D, FF]); inp("moe_w3", [L, NE, D, FF]); inp("moe_w2", [L, NE, FF, D])

        xs = self.scratch("xs", [BL, D, T], F32)
        proj = self.scratch("proj", [BL, NPROJ, T], BF16)
        vtok = self.scratch("vtok", [BL, T, 512], BF16)
        outT = self.scratch("outT", [BL, D, TL], F32, out=True)
        mix = self.scratch("mix", [BL, D, T], BF16)
        self.scratch("nab", [4, 28, 128, 512], F32)
        self.scratch("retw", [4, 38, 128, 512], BF16)
        self.scratch("s5m", [2, 16, 128, 14 * 128], BF16)
        NTmax = BL * 18
        capmax = ((NTmax * 256 + 32 * 127 + 127) // 128) * 128
        self.scratch("hall", [NTmax * 128, D], BF16)
        self.scratch("hbuf", [capmax, D], BF16)
        self.scratch("ybuf", [capmax, D], F32)

        with contextlib.ExitStack() as st:
            arena_t = st.enter_context(nc.sbuf_tensor("arena", [128, 51000], F32))
            self.A = A = Arena(arena_t[:, :], 51000)
            self.banks = [st.enter_context(nc.psum_tensor("bank%d" % i, [128, 512], F32))[:, :] for i in range(8)]
            self.bres = [Res("bank%d" % i) for i in range(8)]
            self.bpos_ = {"g": 0, "acc": 0, "m": 0}

            self.consts()
            self.stage_mod(cc, w_mod, b_mod, g_mix, g_ffn)
            for l in range(NL):
                xsrc = xin if l == 0 else xs
                xres = self.R["xin"] if l == 0 else self.R["xs"]
                self.layer_weights_in(l, w_fm, w_v)
                for b in range(BL):
                    self.stage_inproj(l, b, xsrc, xres, proj, vtok)
                em.barrier()
                A.pop()
                need_ctx = l < DEPTH - 1
                A.push()
                self.mla_weights(l)
                for b in range(BL):
                    self.stage_mla(l, b, need_ctx)
                A.pop()
                self.na_prep(l)
                for b in range(BL):
                    self.stage_na(l, b, need_ctx)
                self.ret_prep(l)
                for b in range(BL):
                    self.stage_ret(l, b, need_ctx)
                self.s5_prep(l)
                A.push()
                self.s5_weights(l)
                for b in range(BL):
                    self.stage_s5(l, b, need_ctx)
                A.pop()
                if self.upto == "mix":
                    continue
                A.push()
                self.moe_layer_state(l, need_ctx)
                for b in range(BL):
                    self.stage_outproj(l, b, need_ctx, xsrc, xres)
                self.stage_moe(l, need_ctx)
                em.barrier()
                A.pop()
            if self.upto != "mix":
                for b in range(BL):
                    self.stage_final(b)
            em.finish()
        return nc

    def bank(self, pool="g"):
        lo, n = {"g": (0, 4), "acc": (4, 2), "m": (6, 2)}[pool]
        i = lo + self.bpos_[pool] % n
        self.bpos_[pool] += 1
        return self.banks[i], self.bres[i]

    def consts(self):
        em, A = self.em, self.A
        self.ones_bf = A.alloc([128, 128], BF16)
        self.ones_f = A.alloc([128, 128], F32)
        self.r_const = Res("const")
        rc = self.r_const
        em.op("dve", lambda e: e.memset(self.ones_bf, 1.0), w=[rc])
        em.op("dve", lambda e: e.memset(self.ones_f, 1.0), w=[rc])
        self.eps_col = A.alloc([128, 2], F32)[:, 0:1]
        em.op("dve", lambda e: e.memset(self.eps_col, EPS), w=[rc])
        self.cst = A.alloc([128, 272], F32)
        em.dma("sp", lambda e: e.dma_start(out=self.cst, in_=self.din["cst"]), r=[], w=[rc])
        self.ident_bf = A.alloc([128, 128], BF16)
        em.op("dve", lambda e: e.tensor_copy(out=self.ident_bf, in_=self.cst[:, 0:128]), r=[rc], w=[rc])
        self.cst2 = A.alloc([128, 322], F32)
        em.dma("sp", lambda e: e.dma_start(out=self.cst2, in_=self.din["cst2"]), r=[], w=[rc])
        self.utri_bf = A.alloc([128, 128], BF16)
        em.op("dve", lambda e: e.tensor_copy(out=self.utri_bf, in_=self.cst2[:, 0:128]), r=[rc], w=[rc])
        self.ln8_col = A.alloc([128, 2], F32)[:, 0:1]
        em.op("dve", lambda e: e.memset(self.ln8_col, float(np.log(0.125))), w=[rc])

    def stage_mod(self, cc, w_mod, b_mod, g_mix, g_ffn):
        em, A, NB1, NL = self.em, self.A, self.NB1, self.NL
        L = DEPTH
        self.mod = A.alloc([128, L, 48, NB1], F32)
        self.gsc1 = A.alloc([128, L, 8, NB1], F32)
        self.gsc2 = A.alloc([128, L, 8, NB1], F32)
        self.r_mod = Res("mod")
        A.push()
        cT = A.alloc([128, 8, NB1], F32)
        cact = A.alloc([128, 8, NB1], F32)
        bmodT = A.alloc([128, L, 48], F32)
        gmixT = A.alloc([128, L, 8], F32)
        gffnT = A.alloc([128, L, 8], F32)
        wbuf = [A.alloc([128, 8, 512], F32) for _ in range(2)]
        wres = [Res("wm0"), Res("wm1")]
        r_c, r_small = Res("cT"), Res("small")
        for b in range(NB1):
            em.dma("sp", lambda e, b=b: e.dma_start(out=cT[:, :, b], in_=cc[b].rearrange("(k p) -> p k", p=128),
                                                    allow_slow_non_contiguous=True), r=[self.R["cc"]], w=[r_c])
        for l in range(L):
            em.dma("sp", lambda e, l=l: e.dma_start(out=bmodT[:, l, :], in_=b_mod[l].rearrange("(f p) -> p f", p=128),
                                                    allow_slow_non_contiguous=True), r=[self.R["b_mod"]], w=[r_small])
            em.dma("sp", lambda e, l=l: e.dma_start(out=gmixT[:, l, :], in_=g_mix[l].rearrange("(f p) -> p f", p=128),
                                                    allow_slow_non_contiguous=True), r=[self.R["g_mix"]], w=[r_small])
            em.dma("sp", lambda e, l=l: e.dma_start(out=gffnT[:, l, :], in_=g_ffn[l].rearrange("(f p) -> p f", p=128),
                                                    allow_slow_non_contiguous=True), r=[self.R["g_ffn"]], w=[r_small])
        em.op("act", lambda e: e.activation(out=cact, in_=cT, func=AF.Silu), r=[r_c], w=[r_c])
        it = 0
        for l in range(NL):
            ps, pr = self.bank()
            for j in range(12):
                wb, wr = wbuf[it % 2], wres[it % 2]
                it += 1
                em.dma("sp", lambda e, wb=wb, l=l, j=j: e.dma_start(
                    out=wb, in_=w_mod[l, :, j * 512:(j + 1) * 512].rearrange("(k p) n -> p k n", p=128)),
                    r=[self.R["w_mod"]], w=[wr])
                for s in range(4):
                    fj = j * 4 + s
                    for k in range(8):
                        em.op("pe", lambda e, wb=wb, s=s, k=k, fj=fj, ps=ps: e.matmul(
                            out=ps[:, fj * NB1:(fj + 1) * NB1], lhsT=wb[:, k, s * 128:(s + 1) * 128],
                            rhs=cact[:, k, :], start=(k == 0), stop=(k == 7)), r=[wr, r_c], w=[pr])
            psv = ps[:, 0:48 * NB1].rearrange("p (f b) -> p f b", b=NB1)
            for b in range(NB1):
                em.op("dve", lambda e, l=l, b=b, psv=psv: e.tensor_tensor(
                    out=self.mod[:, l, :, b], in0=psv[:, :, b], in1=bmodT[:, l, :], op=ALU.add),
                    r=[pr, r_small], w=[self.r_mod])
                em.op("dve", lambda e, l=l, b=b: e.scalar_tensor_tensor(
                    out=self.gsc1[:, l, :, b], in0=self.mod[:, l, 8:16, b], scalar=1.0, in1=gmixT[:, l, :],
                    op0=ALU.add, op1=ALU.mult), r=[r_small], w=[self.r_mod])
                em.op("dve", lambda e, l=l, b=b: e.scalar_tensor_tensor(
                    out=self.gsc2[:, l, :, b], in0=self.mod[:, l, 32:40, b], scalar=1.0, in1=gffnT[:, l, :],
                    op0=ALU.add, op1=ALU.mult), r=[r_small], w=[self.r_mod])
        if "dbg_mod" in self.dbg:
            dm = self.scratch("dbg_mod", [128, L * 48 * NB1], F32)
            em.dma("sp", lambda e: e.dma_start(out=dm, in_=self.mod.rearrange("p l f b -> p (l f b)")),
                   r=[self.r_mod], w=[self.R["dbg_mod"]])
        em.barrier()
        A.pop()

    def layer_weights_in(self, l, w_fm, w_v):
        em, A = self.em, self.A
        A.push()
        self.wfm = A.alloc([128, 8, NPROJ], BF16)
        self.wv = A.alloc([128, 8, 512], BF16)
        self.r_win = Res("win")
        for k in range(8):
            for (c0, cn) in chunks_of(NPROJ, 1216):
                em.dma("pool", lambda e, k=k, c0=c0, cn=cn: e.dma_start(
                    out=self.wfm[:, k, c0:c0 + cn], in_=w_fm[l, k * 128:(k + 1) * 128, c0:c0 + cn]),
                    r=[self.R["w_fm"]], w=[self.r_win])
            em.dma("pool", lambda e, k=k: e.dma_start(
                out=self.wv[:, k, :], in_=w_v[l, k * 128:(k + 1) * 128, :]), r=[self.R["w_v"]], w=[self.r_win])

    def norm_mod_chunk(self, xt, n, h_out, gsc, sh, bcol, rx, rh, tmp, rstd, sq, rtmp, f32_out=None):
        em = self.em
        em.op("act", lambda e: e.activation(out=sq[:, :, 0:n], in_=xt[:, :, 0:n], func=AF.Square), r=[rx], w=[rtmp])
        ps, pr = self.bank()
        for k in range(8):
            em.op("pe", lambda e, k=k: e.matmul(out=ps[:, 0:n], lhsT=self.ones_bf, rhs=sq[:, k, 0:n],
                                                start=(k == 0), stop=(k == 7)), r=[rtmp, self.r_const], w=[pr])
        em.op("act", lambda e: e.activation(out=rstd[:, 0:n], in_=ps[:, 0:n], func=AF.Sqrt, bias=self.eps_col,
                                            scale=1.0 / D), r=[pr, self.r_const], w=[rtmp])
        em.op("dve", lambda e: e.reciprocal(out=rstd[:, 0:n], in_=rstd[:, 0:n]), r=[rtmp], w=[rtmp])
        for k in range(8):
            em.op("dve", lambda e, k=k: e.tensor_tensor(out=tmp[:, k, 0:n], in0=xt[:, k, 0:n], in1=rstd[:, 0:n],
                                                        op=ALU.mult), r=[rx, rtmp], w=[rtmp])
            em.op("act", lambda e, k=k: e.activation(out=h_out[:, k, 0:n], in_=tmp[:, k, 0:n], func=AF.Identity,
                                                     bias=sh[:, k, bcol:bcol + 1], scale=gsc[:, k, bcol:bcol + 1]),
                  r=[rtmp, self.r_mod], w=[rh])
            if f32_out is not None:
                em.op("act", lambda e, k=k: e.activation(out=f32_out[:, k, 0:n], in_=tmp[:, k, 0:n], func=AF.Identity,
                                                         bias=sh[:, k, bcol:bcol + 1], scale=gsc[:, k, bcol:bcol + 1]),
                      r=[rtmp, self.r_mod], w=[rh])

    def stage_inproj(self, l, b, xsrc, xres, proj, vtok):
        em, A, BL = self.em, self.A, self.BL
        A.push()
        if not hasattr(self, "eps_col"):
            pass
        xt = [A.alloc([128, 8, 512], F32) for _ in range(2)]
        rx = [Res("xt0"), Res("xt1")]
        sq = A.alloc([128, 8, 512], BF16)
        tmp = A.alloc([128, 8, 512], F32)
        rstd = A.alloc([128, 512], F32)
        rtmp = Res("tmp")
        h = [A.alloc([128, 8, 512], BF16) for _ in range(2)]
        rh = [Res("h0"), Res("h1")]
        ob = [A.alloc([128, 19, 512], BF16) for _ in range(2)]
        ro = [Res("ob0"), Res("ob1")]
        vb = [A.alloc([128, 512], BF16) for _ in range(2)]
        rv = [Res("vb0"), Res("vb1")]
        xv = xsrc[b].rearrange("(k p) t -> p k t", p=128)
        pv = self.dsc["proj"][b].rearrange("(c p) t -> p c t", p=128)
        vi = 0
        ev = 0
        for ci, (t0, n) in enumerate(TCH):
            xb, rxb, hb, rhb, obb, rob = xt[ci % 2], rx[ci % 2], h[ci % 2], rh[ci % 2], ob[ci % 2], ro[ci % 2]
            em.dma("sp", lambda e, xb=xb, t0=t0, n=n: e.dma_start(out=xb[:, :, 0:n], in_=xv[:, :, t0:t0 + n]),
                   r=[xres], w=[rxb])
            bcol = b if t0 < TL else BL
            self.norm_mod_chunk(xb, n, hb, self.gsc1[:, l], self.mod[:, l, 0:8], bcol, rxb, rhb, tmp, rstd, sq, rtmp)
            for rc in range(19):
                ps, pr = self.bank()
                for k in range(8):
                    em.op("pe", lambda e, k=k, rc=rc, ps=ps, hb=hb, n=n: e.matmul(
                        out=ps[:, 0:n], lhsT=self.wfm[:, k, rc * 128:(rc + 1) * 128], rhs=hb[:, k, 0:n],
                        start=(k == 0), stop=(k == 7)), r=[self.r_win, rhb], w=[pr])
                if ev % 2 == 0:
                    em.op("act", lambda e, rc=rc, ps=ps, obb=obb, n=n: e.activation(
                        out=obb[:, rc, 0:n], in_=ps[:, 0:n], func=AF.Copy), r=[pr], w=[rob])
                else:
                    em.op("dve", lambda e, rc=rc, ps=ps, obb=obb, n=n: e.tensor_copy(
                        out=obb[:, rc, 0:n], in_=ps[:, 0:n]), r=[pr], w=[rob])
                ev += 1
            em.dma("pool", lambda e, obb=obb, t0=t0, n=n: e.dma_start(out=pv[:, :, t0:t0 + n], in_=obb[:, :, 0:n]),
                   r=[rob], w=[self.R["proj"]])
            for tt in range(n // 128):
                ps, pr = self.bank()
                vbb, rvb = vb[vi % 2], rv[vi % 2]
                vi += 1
                for k in range(8):
                    em.op("pe", lambda e, k=k, ps=ps, hb=hb, tt=tt: e.matmul(
                        out=ps[:, :], lhsT=hb[:, k, tt * 128:(tt + 1) * 128], rhs=self.wv[:, k, :],
                        start=(k == 0), stop=(k == 7)), r=[self.r_win, rhb], w=[pr])
                em.op("dve", lambda e, ps=ps, vbb=vbb: e.tensor_copy(out=vbb, in_=ps[:, :]), r=[pr], w=[rvb])
                r0 = t0 + tt * 128
                em.dma("pool", lambda e, vbb=vbb, r0=r0: e.dma_start(out=self.dsc["vtok"][b, r0:r0 + 128, :], in_=vbb),
                       r=[rvb], w=[self.R["vtok"]])
        em.barrier()
        A.pop()


    def attend(self, kT, rk, qT, rq, kd, vfn, rv, vw, jobs, scale, fin):
        em = self.em
        LA = 2
        A = self.A
        A.push()
        NPT = 4
        pts = [A.alloc([128, 512], BF16) for _ in range(NPT)]
        rpts = [Res("pt%d" % i) for i in range(NPT)]
        auxf = [A.alloc([128, 512], F32) for _ in range(3)]
        rauxf = [Res("auxf%d" % i) for i in range(3)]
        tmpf = [A.alloc([128, 512], F32) for _ in range(2)]
        rtmpf = [Res("tmpf%d" % i) for i in range(2)]
        pi = ai = ti = 0
        for job in jobs:
            q0, n, items = job["q0"], job["n"], job["items"]
            acc, racc = self.bank("acc")
            sb = [None] * len(items)
            for i in range(len(items) + LA):
                if i < len(items):
                    kt, mode, aux, raux = items[i]
                    ps, pr = self.bank("g")
                    ab = rab = None
                    if aux is not None:
                        ab, rab = auxf[ai % 3], rauxf[ai % 3]
                        ai += 1
                        if mode == "mul":
                            abv = ab.bitcast(BF16)[:, 0:n]
                        else:
                            abv = ab[:, 0:n]
                        em.dma("sp", lambda e, abv=abv, aux=aux: e.dma_start(out=abv, in_=aux), r=[raux], w=[rab])
                        ab = abv
                    sb[i] = (ps, pr, ab, rab)
                    em.op("pe", lambda e, ps=ps, kt=kt, q0=q0, n=n: e.matmul(
                        out=ps[:, 0:n], lhsT=kT[0:kd, kt * 128:(kt + 1) * 128], rhs=qT[0:kd, q0:q0 + n],
                        start=True, stop=True), r=[rk, rq], w=[pr])
                j = i - LA
                if j >= 0:
                    kt, mode, aux, raux = items[j]
                    ps, pr, ab, rab = sb[j]
                    pt, rp = pts[pi % NPT], rpts[pi % NPT]
                    pi += 1
                    if mode == "exp":
                        em.op("act", lambda e, ps=ps, pt=pt, n=n: e.activation(
                            out=pt[:, 0:n], in_=ps[:, 0:n], func=AF.Exp, scale=scale), r=[pr], w=[rp])
                    elif mode == "bexp":
                        tf, rtf = tmpf[ti % 2], rtmpf[ti % 2]
                        ti += 1
                        em.op("dve", lambda e, ps=ps, tf=tf, ab=ab, n=n: e.scalar_tensor_tensor(
                            out=tf[:, 0:n], in0=ps[:, 0:n], scalar=scale, in1=ab, op0=ALU.mult, op1=ALU.add),
                            r=[pr, rab], w=[rtf])
                        em.op("act", lambda e, tf=tf, pt=pt, n=n: e.activation(
                            out=pt[:, 0:n], in_=tf[:, 0:n], func=AF.Exp), r=[rtf], w=[rp])
                    else:
                        em.op("dve", lambda e, ps=ps, pt=pt, ab=ab, n=n: e.tensor_tensor(
                            out=pt[:, 0:n], in0=ps[:, 0:n], in1=ab, op=ALU.mult), r=[pr, rab], w=[rp])
                    em.op("pe", lambda e, acc=acc, kt=kt, pt=pt, n=n, j=j, m=len(items): e.matmul(
                        out=acc[0:vw, 0:n], lhsT=vfn(kt), rhs=pt[:, 0:n], start=(j == 0), stop=(j == m - 1)),
                        r=[rp, rv], w=[racc])
            fin(job, acc, racc)
        em.barrier()
        A.pop()

    def make_fin_softmax(self, b, rowfn):
        em, A = self.em, self.A
        rec = A.alloc([128, 512], F32)
        bcs = A.alloc([128, 512], F32)
        obs = [A.alloc([128, 512], BF16) for _ in range(2)]
        rrec, rbcs, robs = Res("rec"), Res("bcs"), [Res("ob0"), Res("ob1")]
        cnt = [0]
        mix = self.dsc["mix"]

        def fin(job, acc, racc):
            n, q0 = job["n"], job["q0"]
            ob, rob = obs[cnt[0] % 2], robs[cnt[0] % 2]
            cnt[0] += 1
            em.op("dve", lambda e: e.reciprocal(out=rec[64:65, 0:n], in_=acc[64:65, 0:n]), r=[racc], w=[rrec])
            bc, rbc = self.bank("m")
            em.op("pe", lambda e: e.matmul(out=bc[0:64, 0:n], lhsT=self.ones_f[64:65, 0:64], rhs=rec[64:65, 0:n],
                                           start=True, stop=True), r=[rrec, self.r_const], w=[rbc])
            em.op("act", lambda e: e.activation(out=bcs[0:64, 0:n], in_=bc[0:64, 0:n], func=AF.Copy), r=[rbc], w=[rbcs])
            em.op("dve", lambda e: e.tensor_tensor(out=ob[0:64, 0:n], in0=acc[0:64, 0:n], in1=bcs[0:64, 0:n],
                                                   op=ALU.mult), r=[racc, rbcs], w=[rob])
            row = rowfn(job)
            em.dma("pool", lambda e: e.dma_start(out=mix[b, row:row + 64, q0:q0 + n], in_=ob[0:64, 0:n]),
                   r=[rob], w=[self.R["mix"]])
        return fin

    def mla_weights(self, l):
        em, A = self.em, self.A
        d = self.din
        self.wq_main = [A.alloc([128, 384], BF16) for _ in range(2)]
        self.wq_rot = [A.alloc([128, 4, 96], BF16) for _ in range(2)]
        self.wk = A.alloc([128, 4, 64], BF16)
        self.wvv = A.alloc([128, 4, 64], BF16)
        self.r_mlaw = rw = Res("mlaw")
        A.push()
        f0 = A.alloc([128, 384], F32)
        f1 = A.alloc([128, 384], F32)
        r0 = A.alloc([128, 4, 32], F32)
        r1 = A.alloc([128, 4, 32], F32)
        kvf = A.alloc([128, 512], F32)
        g = A.alloc([128, 4], F32)
        rt = Res("mlatmp")
        em.dma("sp", lambda e: e.dma_start(out=f0, in_=d["mla_w_uq"][l, 0:128, :]), r=[], w=[rt])
        em.dma("sp", lambda e: e.dma_start(out=f1[0:64], in_=d["mla_w_uq"][l, 128:192, :]), r=[], w=[rt])
        em.dma("sp", lambda e: e.dma_start(out=r0, in_=d["w_uq_rot"][l, 0:128]), r=[], w=[rt])
        em.dma("sp", lambda e: e.dma_start(out=r1[0:64], in_=d["w_uq_rot"][l, 128:192]), r=[], w=[rt])
        em.dma("sp", lambda e: e.dma_start(out=kvf, in_=d["mla_w_ukv"][l]), r=[], w=[rt])
        em.dma("sp", lambda e: e.dma_start(out=g[:, 0:1], in_=d["mla_g_cq"][l, 0:128].rearrange("(p o) -> p o", o=1)), r=[], w=[rt])
        em.dma("sp", lambda e: e.dma_start(out=g[0:64, 1:2], in_=d["mla_g_cq"][l, 128:192].rearrange("(p o) -> p o", o=1)), r=[], w=[rt])
        em.dma("sp", lambda e: e.dma_start(out=g[:, 2:3], in_=d["mla_g_ckv"][l].rearrange("(p o) -> p o", o=1)), r=[], w=[rt])
        em.op("dve", lambda e: e.tensor_scalar_mul(out=self.wq_main[0], in0=f0, scalar1=g[:, 0:1]), r=[rt], w=[rw])
        em.op("dve", lambda e: e.tensor_scalar_mul(out=self.wq_main[1][0:64], in0=f1[0:64], scalar1=g[0:64, 1:2]), r=[rt], w=[rw])
        em.op("dve", lambda e: e.memset(self.wq_rot[0], 0.0), w=[rw])
        em.op("dve", lambda e: e.memset(self.wq_rot[1], 0.0), w=[rw])
        em.op("dve", lambda e: e.tensor_scalar_mul(out=self.wq_rot[0][:, :, 64:96], in0=r0, scalar1=g[:, 0:1]), r=[rt], w=[rw])
        em.op("dve", lambda e: e.tensor_scalar_mul(out=self.wq_rot[1][0:64, :, 64:96], in0=r1[0:64], scalar1=g[0:64, 1:2]), r=[rt], w=[rw])
        kv4 = kvf.rearrange("p (h c) -> p h c", h=4)
        em.op("dve", lambda e: e.tensor_scalar_mul(out=self.wk, in0=kv4[:, :, 0:64], scalar1=g[:, 2:3]), r=[rt], w=[rw])
        em.op("dve", lambda e: e.tensor_scalar_mul(out=self.wvv, in0=kv4[:, :, 64:128], scalar1=g[:, 2:3]), r=[rt], w=[rw])
        em.barrier()
        A.pop()

    def stage_mla(self, l, b, need_ctx):
        em, A = self.em, self.A
        d = self.din
        proj, mix = self.dsc["proj"], self.dsc["mix"]
        rproj = self.R["proj"]
        A.push()
        cq0 = A.alloc([128, T], BF16)
        cq1 = A.alloc([128, T], BF16)
        ckv = A.alloc([128, T], BF16)
        kra = A.alloc([128, T], BF16)
        krb = A.alloc([128, T], BF16)
        cosm = A.alloc([128, T], F32)
        sinm = A.alloc([128, T], F32)
        qT = [A.alloc([128, T], BF16) for _ in range(4)]
        kT = [A.alloc([128, T], BF16) for _ in range(4)]
        vaug = A.alloc([128, 18, 4, 65], BF16)
        rin, rtab, rq, rk, rv = Res("mla_in"), Res("mla_tab"), Res("mla_q"), Res("mla_k"), Res("mla_v")
        em.dma("sp", lambda e: e.dma_start(out=cq0, in_=proj[b, 0:128, :]), r=[rproj], w=[rin])
        em.dma("sp", lambda e: e.dma_start(out=cq1[0:64], in_=proj[b, 128:192, :]), r=[rproj], w=[rin])
        em.dma("sp", lambda e: e.dma_start(out=ckv, in_=proj[b, 192:320, :]), r=[rproj], w=[rin])
        em.dma("sp", lambda e: e.dma_start(out=kra[64:96], in_=proj[b, 320:352, :]), r=[rproj], w=[rin])
        em.dma("sp", lambda e: e.dma_start(out=krb[64:96], in_=proj[b, 352:384, :]), r=[rproj], w=[rin])
        em.dma("sp", lambda e: e.dma_start(out=cosm[64:96], in_=d["ropem"][0]), r=[], w=[rtab])
        em.dma("sp", lambda e: e.dma_start(out=sinm[64:96], in_=d["ropem"][1]), r=[], w=[rtab])
        em.op("pool", lambda e: e.memset(vaug, 1.0), w=[rv])
        sq0 = A.alloc([128, 512], BF16)
        sq1 = A.alloc([128, 512], BF16)
        rs = A.alloc([128, 512], F32)
        t1 = A.alloc([128, 512], F32)
        t2 = A.alloc([128, 512], F32)
        rt = Res("mla_t")
        for (t0, n) in TCH:
            sl = slice(t0, t0 + n)
            em.op("act", lambda e, sl=sl, n=n: e.activation(out=sq0[:, 0:n], in_=cq0[:, sl], func=AF.Square), r=[rin], w=[rt])
            em.op("act", lambda e, sl=sl, n=n: e.activation(out=sq1[0:64, 0:n], in_=cq1[0:64, sl], func=AF.Square), r=[rin], w=[rt])
            ps, pr = self.bank("g")
            em.op("pe", lambda e, ps=ps, n=n: e.matmul(out=ps[:, 0:n], lhsT=self.ones_bf, rhs=sq0[:, 0:n], start=True, stop=False), r=[rt, self.r_const], w=[pr])
            em.op("pe", lambda e, ps=ps, n=n: e.matmul(out=ps[:, 0:n], lhsT=self.ones_bf[0:64, :], rhs=sq1[0:64, 0:n], start=False, stop=True), r=[rt], w=[pr])
            em.op("act", lambda e, ps=ps, n=n: e.activation(out=rs[:, 0:n], in_=ps[:, 0:n], func=AF.Sqrt, bias=self.eps_col, scale=1.0 / 192), r=[pr], w=[rt])
            em.op("dve", lambda e, n=n: e.reciprocal(out=rs[:, 0:n], in_=rs[:, 0:n]), r=[rt], w=[rt])
            em.op("dve", lambda e, sl=sl, n=n: e.tensor_tensor(out=cq0[:, sl], in0=cq0[:, sl], in1=rs[:, 0:n], op=ALU.mult), r=[rt], w=[rin])
            em.op("dve", lambda e, sl=sl, n=n: e.tensor_tensor(out=cq1[0:64, sl], in0=cq1[0:64, sl], in1=rs[0:64, 0:n], op=ALU.mult), r=[rt], w=[rin])
            em.op("act", lambda e, sl=sl, n=n: e.activation(out=sq0[:, 0:n], in_=ckv[:, sl], func=AF.Square), r=[rin], w=[rt])
            ps, pr = self.bank("g")
            em.op("pe", lambda e, ps=ps, n=n: e.matmul(out=ps[:, 0:n], lhsT=self.ones_bf, rhs=sq0[:, 0:n], start=True, stop=True), r=[rt, self.r_const], w=[pr])
            em.op("act", lambda e, ps=ps, n=n: e.activation(out=rs[:, 0:n], in_=ps[:, 0:n], func=AF.Sqrt, bias=self.eps_col, scale=1.0 / 128), r=[pr], w=[rt])
            em.op("dve", lambda e, n=n: e.reciprocal(out=rs[:, 0:n], in_=rs[:, 0:n]), r=[rt], w=[rt])
            em.op("dve", lambda e, sl=sl, n=n: e.tensor_tensor(out=ckv[:, sl], in0=ckv[:, sl], in1=rs[:, 0:n], op=ALU.mult), r=[rt], w=[rin])
            em.op("dve", lambda e, sl=sl, n=n: e.tensor_tensor(out=t1[64:96, 0:n], in0=kra[64:96, sl], in1=cosm[64:96, sl], op=ALU.mult), r=[rin, rtab], w=[rt])
            em.op("dve", lambda e, sl=sl, n=n: e.tensor_tensor(out=t2[64:96, 0:n], in0=krb[64:96, sl], in1=sinm[64:96, sl], op=ALU.mult), r=[rin, rtab], w=[rt])
            em.op("dve", lambda e, sl=sl, n=n: e.tensor_tensor(out=kra[64:96, sl], in0=t1[64:96, 0:n], in1=t2[64:96, 0:n], op=ALU.add), r=[rt], w=[rin])
            for h in range(4):
                pa, pra = self.bank("g")
                pb, prb = self.bank("g")
                em.op("pe", lambda e, pa=pa, h=h, sl=sl, n=n: e.matmul(out=pa[0:96, 0:n], lhsT=self.wq_main[0][:, h * 96:(h + 1) * 96], rhs=cq0[:, sl], start=True, stop=False), r=[rin, self.r_mlaw], w=[pra])
                em.op("pe", lambda e, pa=pa, h=h, sl=sl, n=n: e.matmul(out=pa[0:96, 0:n], lhsT=self.wq_main[1][0:64, h * 96:(h + 1) * 96], rhs=cq1[0:64, sl], start=False, stop=True), r=[rin], w=[pra])
                em.op("pe", lambda e, pb=pb, h=h, sl=sl, n=n: e.matmul(out=pb[0:96, 0:n], lhsT=self.wq_rot[0][:, h, :], rhs=cq0[:, sl], start=True, stop=False), r=[rin, self.r_mlaw], w=[prb])
                em.op("pe", lambda e, pb=pb, h=h, sl=sl, n=n: e.matmul(out=pb[0:96, 0:n], lhsT=self.wq_rot[1][0:64, h, :], rhs=cq1[0:64, sl], start=False, stop=True), r=[rin], w=[prb])
                em.op("act", lambda e, pa=pa, h=h, sl=sl, n=n: e.activation(out=qT[h][0:64, sl], in_=pa[0:64, 0:n], func=AF.Copy), r=[pra], w=[rq])
                em.op("dve", lambda e, pa=pa, sl=sl, n=n: e.tensor_tensor(out=t1[64:96, 0:n], in0=pa[64:96, 0:n], in1=cosm[64:96, sl], op=ALU.mult), r=[pra, rtab], w=[rt])
                em.op("dve", lambda e, pb=pb, sl=sl, n=n: e.tensor_tensor(out=t2[64:96, 0:n], in0=pb[64:96, 0:n], in1=sinm[64:96, sl], op=ALU.mult), r=[prb, rtab], w=[rt])
                em.op("dve", lambda e, h=h, sl=sl, n=n: e.tensor_tensor(out=qT[h][64:96, sl], in0=t1[64:96, 0:n], in1=t2[64:96, 0:n], op=ALU.add), r=[rt], w=[rq])
                pk, prk = self.bank("g")
                em.op("pe", lambda e, pk=pk, h=h, sl=sl, n=n: e.matmul(out=pk[0:64, 0:n], lhsT=self.wk[:, h, :], rhs=ckv[:, sl], start=True, stop=True), r=[rin, self.r_mlaw], w=[prk])
                em.op("act", lambda e, pk=pk, h=h, sl=sl, n=n: e.activation(out=kT[h][0:64, sl], in_=pk[0:64, 0:n], func=AF.Copy), r=[prk], w=[rk])
                em.op("act", lambda e, h=h, sl=sl: e.activation(out=kT[h][64:96, sl], in_=kra[64:96, sl], func=AF.Copy), r=[rin], w=[rk])
            for tt in range(n // 128):
                kt = t0 // 128 + tt
                pv, prv = self.bank("g")
                em.op("pe", lambda e, pv=pv, kt=kt: e.matmul(out=pv[:, 0:256], lhsT=ckv[:, kt * 128:(kt + 1) * 128], rhs=self.wvv.rearrange("p h c -> p (h c)"), start=True, stop=True), r=[rin, self.r_mlaw], w=[prv])
                em.op("dve", lambda e, pv=pv, kt=kt: e.tensor_copy(out=vaug[:, kt, :, 0:64], in_=pv[:, 0:256].rearrange("p (h c) -> p h c", h=4)), r=[prv], w=[rv])
        scale = 96.0 ** -0.5
        for h in range(4):
            jobs = [dict(q0=q0, n=512, items=[(kt, "exp", None, None) for kt in range(18)], h=h) for q0 in (0, 512, 1024, 1536)]
            if need_ctx:
                jobs.append(dict(q0=2048, n=256, items=[(16, "exp", None, None), (17, "exp", None, None)], h=h))
            A.push()
            fin = self.make_fin_softmax(b, lambda job: job["h"] * 64)
            self.attend(kT[h], rk, qT[h], rq, 96, lambda kt, h=h: vaug[:, kt, h, :], rv, 65, jobs, scale, fin)
            A.pop()
        em.barrier()
        A.pop()


    NA_PAIRS = [(0, kt) for kt in range(0, 6)] + [(1, kt) for kt in range(2, 10)] + \
               [(2, kt) for kt in range(6, 14)] + [(3, kt) for kt in range(10, 16)]

    def na_prep(self, l):
        em, A = self.em, self.A
        d = self.din
        nab = self.dsc["nab"]
        A.push()
        tb = [A.alloc([128, 512], F32) for _ in range(3)]
        mb = [A.alloc([128, 512], F32) for _ in range(3)]
        rtb = [Res("natb%d" % i) for i in range(3)]
        rmb = [Res("namb%d" % i) for i in range(3)]
        it = 0
        for h in range(4):
            for pi, (qi, kt) in enumerate(self.NA_PAIRS):
                t, rt, m, rm = tb[it % 3], rtb[it % 3], mb[it % 3], rmb[it % 3]
                it += 1
                for krl in range(2):
                    j0 = 18 - (2 * kt + krl - 8 * qi + 7)
                    em.dma("sp", lambda e, t=t, krl=krl, j0=j0, h=h: e.dma_start(
                        out=t[krl * 64:(krl + 1) * 64, :].rearrange("k (j q) -> k j q", j=8),
                        in_=d["na_tc"][l, h, j0:j0 + 8].rearrange("j k q -> k j q")), r=[], w=[rt])
                em.dma("sp", lambda e, m=m, pi=pi: e.dma_start(out=m, in_=d["na_mask"][pi]), r=[], w=[rm])
                em.op("pool", lambda e, t=t, m=m: e.tensor_tensor(out=t, in0=t, in1=m, op=ALU.add), r=[rm, rt], w=[rt])
                em.dma("pool", lambda e, t=t, h=h, pi=pi: e.dma_start(out=nab[h, pi], in_=t), r=[rt], w=[self.R["nab"]])
        em.barrier()
        A.pop()

    def load_vaug(self, b, c0, vw, rv):
        em, A = self.em, self.A
        vaug = A.alloc([128, 18, 4, vw], BF16)
        if vw == 65:
            em.op("pool", lambda e: e.memset(vaug, 1.0), w=[rv])
        vt = self.dsc["vtok"]
        for h in range(4):
            em.dma("sp", lambda e, h=h: e.dma_start(
                out=vaug[:, :, h, 0:64], in_=vt[b, :, c0 + h * 64:c0 + (h + 1) * 64].rearrange("(k p) c -> p k c", p=128)),
                r=[self.R["vtok"]], w=[rv])
        return vaug

    def stage_na(self, l, b, need_ctx):
        em, A = self.em, self.A
        proj, nab = self.dsc["proj"], self.dsc["nab"]
        rproj = self.R["proj"]
        A.push()
        rq, rk, rv = Res("na_q"), Res("na_k"), Res("na_v")
        qT = [A.alloc([128, T], BF16) for _ in range(4)]
        kT = [A.alloc([128, T], BF16) for _ in range(4)]
        for h in range(4):
            em.dma("sp", lambda e, h=h: e.dma_start(out=qT[h][0:64], in_=proj[b, R_NAQ + h * 64:R_NAQ + (h + 1) * 64, :]), r=[rproj], w=[rq])
            em.dma("sp", lambda e, h=h: e.dma_start(out=kT[h][0:64], in_=proj[b, R_NAK + h * 64:R_NAK + (h + 1) * 64, :]), r=[rproj], w=[rk])
        vaug = self.load_vaug(b, 0, 65, rv)
        for h in range(4):
            jobs = []
            for qi in range(4):
                items = [(kt, "bexp", nab[h, pi], self.R["nab"]) for pi, (q2, kt) in enumerate(self.NA_PAIRS) if q2 == qi]
                items += [(16, "exp", None, None), (17, "exp", None, None)]
                jobs.append(dict(q0=qi * 512, n=512, items=items, h=h))
            if need_ctx:
                jobs.append(dict(q0=2048, n=256, items=[(16, "exp", None, None), (17, "exp", None, None)], h=h))
            A.push()
            fin = self.make_fin_softmax(b, lambda job: 256 + job["h"] * 64)
            self.attend(kT[h], rk, qT[h], rq, 64, lambda kt, h=h: vaug[:, kt, h, :], rv, 65, jobs, 0.125, fin)
            A.pop()
        em.barrier()
        A.pop()

    def ret_prep(self, l):
        em, A = self.em, self.A
        d = self.din
        retw = self.dsc["retw"]
        A.push()
        lg = A.alloc([128, 8], F32)
        rlg = Res("lg")
        em.dma("sp", lambda e: e.dma_start(out=lg, in_=d["ret_log_decay"][l].rearrange("d h -> (d h)").partition_broadcast(128)), r=[], w=[rlg])
        df = [A.alloc([128, 512], F32) for _ in range(2)]
        db = [A.alloc([128, 512], F32) for _ in range(2)]
        wt = [A.alloc([128, 512], BF16) for _ in range(2)]
        rdf = [Res("df0"), Res("df1")]
        rdb = [Res("db0"), Res("db1")]
        rwt = [Res("wt0"), Res("wt1")]
        it = 0
        for h in range(4):
            for tid in range(38):
                i = it % 2
                it += 1
                em.dma("sp", lambda e, i=i, tid=tid: e.dma_start(out=df[i], in_=d["retd"][tid, 0]), r=[], w=[rdf[i]])
                em.dma("sp", lambda e, i=i, tid=tid: e.dma_start(out=db[i], in_=d["retd"][tid, 1]), r=[], w=[rdb[i]])
                em.op("act", lambda e, i=i, h=h: e.activation(out=df[i], in_=df[i], func=AF.Exp, bias=self.ln8_col, scale=lg[:, h:h + 1]), r=[rlg, self.r_const], w=[rdf[i]])
                em.op("act", lambda e, i=i, h=h: e.activation(out=db[i], in_=db[i], func=AF.Exp, bias=self.ln8_col, scale=lg[:, 4 + h:5 + h]), r=[rlg, self.r_const], w=[rdb[i]])
                em.op("dve", lambda e, i=i: e.tensor_tensor(out=wt[i], in0=df[i], in1=db[i], op=ALU.add), r=[rdf[i], rdb[i]], w=[rwt[i]])
                em.dma("pool", lambda e, i=i, h=h, tid=tid: e.dma_start(out=retw[h, tid], in_=wt[i]), r=[rwt[i]], w=[self.R["retw"]])
        em.barrier()
        A.pop()

    def stage_ret(self, l, b, need_ctx):
        em, A = self.em, self.A
        d = self.din
        proj, retw, mix = self.dsc["proj"], self.dsc["retw"], self.dsc["mix"]
        rproj = self.R["proj"]
        A.push()
        rv, rtab = Res("ret_v"), Res("ret_tab")
        cosr = A.alloc([128, T], F32)
        sinr = A.alloc([128, T], F32)
        em.dma("sp", lambda e: e.dma_start(out=cosr[0:64], in_=d["roper"][0]), r=[], w=[rtab])
        em.dma("sp", lambda e: e.dma_start(out=sinr[0:64], in_=d["roper"][1]), r=[], w=[rtab])
        vr = self.load_vaug(b, 256, 64, rv)
        ra = A.alloc([128, T], BF16)
        rb = A.alloc([128, T], BF16)
        qT = A.alloc([128, T], BF16)
        kT = A.alloc([128, T], BF16)
        gT = A.alloc([128, T], BF16)
        t1 = A.alloc([128, 512], F32)
        t2 = A.alloc([128, 512], F32)
        ysb = A.alloc([128, 512], F32)
        cen = A.alloc([128, 512], F32)
        sqf = A.alloc([128, 512], F32)
        rsd = A.alloc([128, 512], F32)
        sg = A.alloc([128, 512], F32)
        obs = [A.alloc([128, 512], BF16) for _ in range(2)]
        robs = [Res("rob0"), Res("rob1")]
        rin, rq, rk, rg, rt, rf = Res("ret_in"), Res("ret_q"), Res("ret_k"), Res("ret_g"), Res("ret_t"), Res("ret_f")
        cnt = [0]
        for h in range(4):
            for (base, basep, dst, rdst) in ((R_RQ, R_RQP, qT, rq), (R_RK, R_RKP, kT, rk)):
                em.dma("sp", lambda e, base=base, h=h: e.dma_start(out=ra[0:64], in_=proj[b, base + h * 64:base + (h + 1) * 64, :]), r=[rproj], w=[rin])
                em.dma("sp", lambda e, basep=basep, h=h: e.dma_start(out=rb[0:64], in_=proj[b, basep + h * 64:basep + (h + 1) * 64, :]), r=[rproj], w=[rin])
                for (t0, n) in TCH:
                    sl = slice(t0, t0 + n)
                    em.op("dve", lambda e, sl=sl, n=n: e.tensor_tensor(out=t1[0:64, 0:n], in0=ra[0:64, sl], in1=cosr[0:64, sl], op=ALU.mult), r=[rin, rtab], w=[rt])
                    em.op("pool", lambda e, sl=sl, n=n: e.tensor_tensor(out=t2[0:64, 0:n], in0=rb[0:64, sl], in1=sinr[0:64, sl], op=ALU.mult), r=[rin, rtab], w=[rf])
                    em.op("dve", lambda e, sl=sl, n=n, dst=dst: e.tensor_tensor(out=dst[0:64, sl], in0=t1[0:64, 0:n], in1=t2[0:64, 0:n], op=ALU.add), r=[rt, rf], w=[rdst])
            em.dma("sp", lambda e, h=h: e.dma_start(out=gT[0:64], in_=proj[b, R_RG + h * 64:R_RG + (h + 1) * 64, :]), r=[rproj], w=[rg])
            jobs = []
            for qi in range(4):
                items = [(kt, "mul", retw[h, (512 * qi - 128 * kt + 1920) // 128], self.R["retw"]) for kt in range(16)]
                items += [(16 + c, "mul", retw[h, 28 + qi * 2 + c], self.R["retw"]) for c in range(2)]
                jobs.append(dict(q0=qi * 512, n=512, items=items, h=h))
            if need_ctx:
                jobs.append(dict(q0=2048, n=256, items=[(16 + c, "mul", retw[h, 36 + c][:, 0:256], self.R["retw"]) for c in range(2)], h=h))

            def fin(job, acc, racc):
                n, q0, hh = job["n"], job["q0"], job["h"]
                ob, rob = obs[cnt[0] % 2], robs[cnt[0] % 2]
                cnt[0] += 1
                onesl = self.ones_f[0:64, 0:64]
                em.op("act", lambda e: e.activation(out=ysb[0:64, 0:n], in_=acc[0:64, 0:n], func=AF.Copy), r=[racc], w=[rf])
                p1, pr1 = self.bank("m")
                em.op("pe", lambda e: e.matmul(out=p1[0:64, 0:n], lhsT=onesl, rhs=ysb[0:64, 0:n], start=True, stop=True), r=[rf, self.r_const], w=[pr1])
                em.op("dve", lambda e: e.scalar_tensor_tensor(out=cen[0:64, 0:n], in0=p1[0:64, 0:n], scalar=-1.0 / 64, in1=ysb[0:64, 0:n], op0=ALU.mult, op1=ALU.add), r=[pr1, rf], w=[rf])
                em.op("act", lambda e: e.activation(out=sqf[0:64, 0:n], in_=cen[0:64, 0:n], func=AF.Square), r=[rf], w=[rf])
                p2, pr2 = self.bank("m")
                em.op("pe", lambda e: e.matmul(out=p2[0:64, 0:n], lhsT=onesl, rhs=sqf[0:64, 0:n], start=True, stop=True), r=[rf], w=[pr2])
                em.op("act", lambda e: e.activation(out=rsd[0:64, 0:n], in_=p2[0:64, 0:n], func=AF.Sqrt, bias=self.eps_col[0:64], scale=1.0 / 64), r=[pr2], w=[rf])
                em.op("dve", lambda e: e.reciprocal(out=rsd[0:64, 0:n], in_=rsd[0:64, 0:n]), r=[rf], w=[rf])
                em.op("act", lambda e: e.activation(out=sg[0:64, 0:n], in_=gT[0:64, q0:q0 + n], func=AF.Silu), r=[rg], w=[rf])
                em.op("dve", lambda e: e.tensor_tensor(out=cen[0:64, 0:n], in0=cen[0:64, 0:n], in1=rsd[0:64, 0:n], op=ALU.mult), r=[rf], w=[rf])
                em.op("dve", lambda e: e.tensor_tensor(out=ob[0:64, 0:n], in0=cen[0:64, 0:n], in1=sg[0:64, 0:n], op=ALU.mult), r=[rf], w=[rob])
                row = 768 + hh * 64
                em.dma("pool", lambda e: e.dma_start(out=mix[b, row:row + 64, q0:q0 + n], in_=ob[0:64, 0:n]), r=[rob], w=[self.R["mix"]])

            self.attend(kT, rk, qT, rq, 64, lambda kt, h=h: vr[:, kt, h, :], rv, 64, jobs, 1.0, fin)
        em.barrier()
        A.pop()


    def s5_prep(self, l):
        em, A = self.em, self.A
        d = self.din
        s5m = self.dsc["s5m"]
        A.push()
        PI = float(np.pi)
        rp, rb, rm = Res("s5p"), Res("s5b"), Res("s5mat")
        are, aim, dtt = A.alloc([128, 16], F32), A.alloc([128, 16], F32), A.alloc([128, 16], F32)
        ldr, ldi, mag, w1, w2 = (A.alloc([128, 16], F32) for _ in range(5))
        sn, cs, den, nr, f1, f2, q1, q2 = (A.alloc([128, 16], F32) for _ in range(8))
        A1 = A.alloc([128, 12, 16], F32)
        A2 = A.alloc([128, 12, 16], F32)
        ki = A.alloc([128, 16], I32)
        X1 = A.alloc([128, 16, 16], F32)
        X2 = A.alloc([128, 16, 16], F32)
        BB = A.alloc([128, 16, 16], F32)
        BT = A.alloc([128, 16, 16], F32)
        CC = A.alloc([128, 128], F32)
        cp8 = A.alloc([128, 8, 128], BF16)
        wb8 = A.alloc([128, 8, 128], BF16)
        tmpM = [A.alloc([128, 128], F32) for _ in range(2)]
        Mb = [A.alloc([128, 12, 128], BF16) for _ in range(2)]
        rMb = [Res("Mb0"), Res("Mb1")]
        rtm = [Res("tm0"), Res("tm1")]
        sgn = self.cst[:, 264:265]
        for dd in range(2):
            for half in range(2):
                ps_ = slice(half * 64, half * 64 + 64)
                em.dma("sp", lambda e, ps_=ps_, dd=dd: e.dma_start(out=are[ps_], in_=d["s5_a_re"][l, dd].rearrange("g p -> p g"), allow_slow_non_contiguous=True), r=[], w=[rp])
                em.dma("sp", lambda e, ps_=ps_, dd=dd: e.dma_start(out=aim[ps_], in_=d["s5_a_im"][l, dd].rearrange("g p -> p g"), allow_slow_non_contiguous=True), r=[], w=[rp])
            em.dma("sp", lambda e, dd=dd: e.dma_start(out=dtt, in_=d["s5_log_dt"][l, dd].partition_broadcast(128)), r=[], w=[rp])
            em.dma("sp", lambda e, dd=dd: e.dma_start(out=X1[0:64], in_=d["s5_b_re"][l, dd].rearrange("g p c -> p g c")), r=[], w=[rb])
            em.dma("sp", lambda e, dd=dd: e.dma_start(out=X1[64:128], in_=d["s5_b_im"][l, dd].rearrange("g p c -> p g c")), r=[], w=[rb])
            em.dma("sp", lambda e, dd=dd: e.dma_start(out=X2[0:64], in_=d["s5_b_im"][l, dd].rearrange("g p c -> p g c")), r=[], w=[rb])
            em.dma("sp", lambda e, dd=dd: e.dma_start(out=X2[64:128], in_=d["s5_b_re"][l, dd].rearrange("g p c -> p g c")), r=[], w=[rb])
            V = lambda fn, **kw: em.op("dve", fn, r=[rp, self.r_const], w=[rp])
            em.op("act", lambda e: e.activation(out=dtt, in_=dtt, func=AF.Exp), r=[rp], w=[rp])
            V(lambda e: e.tensor_tensor(out=ldr, in0=are, in1=dtt, op=ALU.mult))
            V(lambda e: e.tensor_tensor(out=ldi, in0=aim, in1=dtt, op=ALU.mult))
            em.op("act", lambda e: e.activation(out=mag, in_=ldr, func=AF.Exp), r=[rp], w=[rp])
            for (wx, off, dst) in ((w1, 8.0, sn), (w2, 8.25, cs)):
                V(lambda e, wx=wx, off=off: e.tensor_scalar(out=wx, in0=ldi, scalar1=1.0 / (2.0 * PI), scalar2=off, op0=ALU.mult, op1=ALU.add))
                V(lambda e, wx=wx: e.tensor_copy(out=ki, in_=wx))
                V(lambda e: e.tensor_copy(out=q1, in_=ki))
                V(lambda e, wx=wx: e.tensor_tensor(out=wx, in0=wx, in1=q1, op=ALU.subtract))
                V(lambda e, wx=wx: e.tensor_scalar(out=q2, in0=wx, scalar1=0.5, scalar2=None, op0=ALU.is_gt))
                V(lambda e, wx=wx: e.tensor_tensor(out=wx, in0=wx, in1=q2, op=ALU.subtract))
                em.op("act", lambda e, wx=wx, dst=dst: e.activation(out=dst, in_=wx, func=AF.Sin, scale=2.0 * PI), r=[rp], w=[rp])
            V(lambda e: e.tensor_tensor(out=A1[:, 0, :], in0=mag, in1=cs, op=ALU.mult))
            V(lambda e: e.tensor_tensor(out=sn, in0=mag, in1=sn, op=ALU.mult))
            V(lambda e: e.tensor_scalar(out=A2[:, 0, :], in0=sn, scalar1=sgn, scalar2=None, op0=ALU.mult))
            V(lambda e: e.tensor_scalar(out=nr, in0=A1[:, 0, :], scalar1=-1.0, scalar2=None, op0=ALU.add))
            V(lambda e: e.tensor_tensor(out=q1, in0=are, in1=are, op=ALU.mult))
            V(lambda e: e.tensor_tensor(out=q2, in0=aim, in1=aim, op=ALU.mult))
            V(lambda e: e.tensor_tensor(out=den, in0=q1, in1=q2, op=ALU.add))
            V(lambda e: e.reciprocal(out=den, in_=den))
            V(lambda e: e.tensor_tensor(out=q1, in0=nr, in1=are, op=ALU.mult))
            V(lambda e: e.tensor_tensor(out=q2, in0=sn, in1=aim, op=ALU.mult))
            V(lambda e: e.tensor_tensor(out=q1, in0=q1, in1=q2, op=ALU.add))
            V(lambda e: e.tensor_tensor(out=f1, in0=q1, in1=den, op=ALU.mult))
            V(lambda e: e.tensor_tensor(out=q1, in0=sn, in1=are, op=ALU.mult))
            V(lambda e: e.tensor_tensor(out=q2, in0=nr, in1=aim, op=ALU.mult))
            V(lambda e: e.tensor_tensor(out=q1, in0=q1, in1=q2, op=ALU.subtract))
            V(lambda e: e.tensor_tensor(out=q1, in0=q1, in1=den, op=ALU.mult))
            V(lambda e: e.tensor_scalar(out=f2, in0=q1, scalar1=sgn, scalar2=-1.0, op0=ALU.mult, op1=ALU.mult))
            for k in range(1, 12):
                V(lambda e, k=k: e.tensor_tensor(out=q1, in0=A1[:, k - 1, :], in1=A1[:, k - 1, :], op=ALU.mult))
                V(lambda e, k=k: e.tensor_tensor(out=q2, in0=A2[:, k - 1, :], in1=A2[:, k - 1, :], op=ALU.mult))
                V(lambda e, k=k: e.tensor_tensor(out=A1[:, k, :], in0=q1, in1=q2, op=ALU.subtract))
                V(lambda e, k=k: e.scalar_tensor_tensor(out=A2[:, k, :], in0=A1[:, k - 1, :], scalar=2.0, in1=A2[:, k - 1, :], op0=ALU.mult, op1=ALU.mult))
            if "dbg_s5" in self.dbg and dd == 0:
                dbg = self.scratch("dbg_s5", [128, 14, 16], F32)
                for i_, t_ in enumerate([are, aim, dtt, ldr, ldi, mag, w1, w2, sn, cs, A1[:, 0, :], A2[:, 0, :], f1, f2]):
                    em.dma("sp", lambda e, i_=i_, t_=t_: e.dma_start(out=dbg[:, i_, :], in_=t_), r=[rp], w=[self.R["dbg_s5"]])
                em.barrier()
            em.op("dve", lambda e: e.tensor_tensor(out=BB, in0=X1, in1=f1.unsqueeze(2).to_broadcast([128, 16, 16]), op=ALU.mult), r=[rp, rb], w=[rb])
            em.op("dve", lambda e: e.tensor_tensor(out=BT, in0=X2, in1=f2.unsqueeze(2).to_broadcast([128, 16, 16]), op=ALU.mult), r=[rp, rb], w=[rb])
            em.op("dve", lambda e: e.tensor_tensor(out=BB, in0=BB, in1=BT, op=ALU.add), r=[rb], w=[rb])
            for j in range(2):
                pt, prt = self.bank("m")
                em.op("pe", lambda e, pt=pt, j=j: e.transpose(out=pt[:, 0:128], in_=BB[:, 8 * j:8 * j + 8, :].rearrange("p g c -> p (g c)"), identity=self.cst[:, 0:128]), r=[rb, self.r_const], w=[prt])
                for gl in range(8):
                    em.op("dve", lambda e, pt=pt, gl=gl: e.tensor_scalar(out=wb8[:, gl, :], in0=pt[:, 0:128], scalar1=self.cst[:, 256 + gl:257 + gl], scalar2=None, op0=ALU.mult), r=[prt, self.r_const], w=[rm])
                em.dma("pool", lambda e, dd=dd, j=j: e.dma_start(out=s5m[dd, 8 * j:8 * j + 8, :, 12 * 128:13 * 128].rearrange("g p c -> p g c"), in_=wb8), r=[rm], w=[self.R["s5m"]])
                em.dma("sp", lambda e, dd=dd, j=j: e.dma_start(out=CC[:, 0:64], in_=d["s5_c_re"][l, dd, 8 * j:8 * j + 8].rearrange("g c p -> (g c) p")), r=[], w=[rm])
                em.dma("sp", lambda e, dd=dd, j=j: e.dma_start(out=CC[:, 64:128], in_=d["s5_c_im"][l, dd, 8 * j:8 * j + 8].rearrange("g c p -> (g c) p")), r=[], w=[rm])
                em.op("act", lambda e: e.mul(out=CC[:, 64:128], in_=CC[:, 64:128], mul=-1.0), r=[rm], w=[rm])
                pt2, prt2 = self.bank("m")
                em.op("pe", lambda e, pt2=pt2: e.transpose(out=pt2[:, 0:128], in_=CC, identity=self.cst[:, 0:128]), r=[rm, self.r_const], w=[prt2])
                em.op("pool", lambda e: e.memset(cp8, 0.0), w=[rm])
                for gl in range(8):
                    em.op("act", lambda e, pt2=pt2, gl=gl: e.activation(out=cp8[:, gl, 16 * gl:16 * gl + 16], in_=pt2[:, 16 * gl:16 * gl + 16], func=AF.Copy), r=[prt2], w=[rm])
                em.dma("pool", lambda e, dd=dd, j=j: e.dma_start(out=s5m[dd, 8 * j:8 * j + 8, :, 13 * 128:14 * 128].rearrange("g p c -> p g c"), in_=cp8), r=[rm], w=[self.R["s5m"]])
            it = 0
            for g in range(16):
                mb, rmb = Mb[g % 2], rMb[g % 2]
                for k in range(12):
                    tm, rt_ = tmpM[it % 2], rtm[it % 2]
                    it += 1
                    eng = "dve"
                    em.op(eng, lambda e, tm=tm, k=k, g=g: e.tensor_scalar(out=tm, in0=self.cst[:, 0:128], scalar1=A1[:, k, g:g + 1], scalar2=None, op0=ALU.mult), r=[rp, self.r_const], w=[rt_])
                    em.op(eng, lambda e, tm=tm, k=k, g=g, mb=mb: e.scalar_tensor_tensor(out=mb[:, k, :], in0=self.cst[:, 128:256], scalar=A2[:, k, g:g + 1], in1=tm, op0=ALU.mult, op1=ALU.add), r=[rp, rt_, self.r_const], w=[rmb])
                em.dma("pool", lambda e, mb=mb, dd=dd, g=g: e.dma_start(out=s5m[dd, g, :, 0:12 * 128], in_=mb.rearrange("p k c -> p (k c)")), r=[rmb], w=[self.R["s5m"]])
        em.barrier()
        A.pop()

    def s5_weights(self, l):
        em, A = self.em, self.A
        d = self.din
        self.wglu = A.alloc([128, 2, 256], BF16)
        self.s5cols = A.alloc([128, 4], F32)
        self.r_s5w = rw = Res("s5w")
        for j in range(2):
            em.dma("pool", lambda e, j=j: e.dma_start(out=self.wglu[:, j, :], in_=d["s5_w_glu"][l, j * 128:(j + 1) * 128, :]), r=[], w=[rw])
            em.dma("sp", lambda e, j=j: e.dma_start(out=self.s5cols[:, j:j + 1], in_=d["s5_d"][l, j * 128:(j + 1) * 128].rearrange("(p o) -> p o", o=1)), r=[], w=[rw])
            em.dma("sp", lambda e, j=j: e.dma_start(out=self.s5cols[:, 2 + j:3 + j], in_=d["s5_b_glu"][l, j * 128:(j + 1) * 128].rearrange("(p o) -> p o", o=1)), r=[], w=[rw])

    def stage_s5(self, l, b, need_ctx):
        em, A = self.em, self.A
        proj, s5m, mix = self.dsc["proj"], self.dsc["s5m"], self.dsc["mix"]
        rproj = self.R["proj"]
        A.push()
        ru, ry, rg = Res("s5u"), Res("s5y"), Res("s5g")
        ub = [A.alloc([128, T], BF16) for _ in range(2)]
        uf = [A.alloc([128, T], BF16) for _ in range(2)]
        yacc = A.alloc([128, 2, T], F32)
        for j in range(2):
            r0 = R_S5 + j * 128
            em.dma("sp", lambda e, j=j, r0=r0: e.dma_start(out=ub[j], in_=proj[b, r0:r0 + 128, :]), r=[rproj], w=[ru])
            em.dma("sp", lambda e, j=j, r0=r0: e.dma_start(out=uf[j][:, 0:TC], in_=proj[b, r0:r0 + 128, TL:T]), r=[rproj], w=[ru])
            em.dma("sp", lambda e, j=j, r0=r0: e.dma_start(out=uf[j][:, TC:T], in_=proj[b, r0:r0 + 128, 0:TL]), r=[rproj], w=[ru])
        em.op("pool", lambda e: e.memset(yacc, 0.0), w=[ry])
        hA = A.alloc([128, T], BF16)
        hB = A.alloc([128, T], BF16)
        rh = [Res("hA"), Res("hB")]
        hb_ = [hA, hB]
        Mg = [A.alloc([128, 14, 128], BF16) for _ in range(2)]
        rMg = [Res("Mg0"), Res("Mg1")]
        it = 0
        for dd in range(2):
            usrc = uf if dd == 0 else ub
            for g in range(16):
                j = g // 8
                mg, rmg = Mg[it % 2], rMg[it % 2]
                it += 1
                em.dma("sp", lambda e, mg=mg, dd=dd, g=g: e.dma_start(out=mg.rearrange("p k c -> p (k c)"), in_=s5m[dd, g]), r=[self.R["s5m"]], w=[rmg])
                cur = 0
                for (c0, n) in chunks_of(T, 512):
                    ps, pr = self.bank("g")
                    em.op("pe", lambda e, ps=ps, mg=mg, c0=c0, n=n, j=j, usrc=usrc: e.matmul(out=ps[:, 0:n], lhsT=mg[:, 12, :], rhs=usrc[j][:, c0:c0 + n], start=True, stop=True), r=[rmg, ru], w=[pr])
                    em.op("act", lambda e, ps=ps, c0=c0, n=n: e.activation(out=hb_[0][:, c0:c0 + n], in_=ps[:, 0:n], func=AF.Copy), r=[pr], w=[rh[0]])
                for k in range(12):
                    dl = 1 << k
                    src, dst, rs_, rd_ = hb_[cur], hb_[1 - cur], rh[cur], rh[1 - cur]
                    if dd == 0:
                        em.op("act", lambda e, src=src, dst=dst, dl=dl: e.activation(out=dst[:, 0:dl], in_=src[:, 0:dl], func=AF.Copy), r=[rs_], w=[rd_])
                        rng = chunks_of(T - dl, 512)
                        for (o, n) in rng:
                            c0 = dl + o
                            ps, pr = self.bank("g")
                            em.op("pe", lambda e, ps=ps, mg=mg, k=k, src=src, c0=c0, n=n, dl=dl: e.matmul(out=ps[:, 0:n], lhsT=mg[:, k, :], rhs=src[:, c0 - dl:c0 - dl + n], start=True, stop=True), r=[rmg, rs_], w=[pr])
                            em.op("dve", lambda e, ps=ps, src=src, dst=dst, c0=c0, n=n: e.tensor_tensor(out=dst[:, c0:c0 + n], in0=ps[:, 0:n], in1=src[:, c0:c0 + n], op=ALU.add), r=[pr, rs_], w=[rd_])
                    else:
                        em.op("act", lambda e, src=src, dst=dst, dl=dl: e.activation(out=dst[:, T - dl:T], in_=src[:, T - dl:T], func=AF.Copy), r=[rs_], w=[rd_])
                        for (c0, n) in chunks_of(T - dl, 512):
                            ps, pr = self.bank("g")
                            em.op("pe", lambda e, ps=ps, mg=mg, k=k, src=src, c0=c0, n=n, dl=dl: e.matmul(out=ps[:, 0:n], lhsT=mg[:, k, :], rhs=src[:, c0 + dl:c0 + dl + n], start=True, stop=True), r=[rmg, rs_], w=[pr])
                            em.op("dve", lambda e, ps=ps, src=src, dst=dst, c0=c0, n=n: e.tensor_tensor(out=dst[:, c0:c0 + n], in0=ps[:, 0:n], in1=src[:, c0:c0 + n], op=ALU.add), r=[pr, rs_], w=[rd_])
                    cur = 1 - cur
                hfin, rhf = hb_[cur], rh[cur]
                if dd == 0:
                    segs = [(0, 256, 2048), (256, 512, 0), (768, 512, 512), (1280, 512, 1024), (1792, 512, 1536)]
                else:
                    segs = [(c0, n, c0) for (c0, n) in chunks_of(T, 512)]
                for (f0, n, s0) in segs:
                    ps, pr = self.bank("g")
                    em.op("pe", lambda e, ps=ps, mg=mg, f0=f0, n=n, hfin=hfin: e.matmul(out=ps[:, 0:n], lhsT=mg[:, 13, :], rhs=hfin[:, f0:f0 + n], start=True, stop=True), r=[rmg, rhf], w=[pr])
                    em.op("dve", lambda e, ps=ps, j=j, s0=s0, n=n: e.tensor_tensor(out=yacc[:, j, s0:s0 + n], in0=ps[:, 0:n], in1=yacc[:, j, s0:s0 + n], op=ALU.add), r=[pr], w=[ry])
        gT = [A.alloc([128, T], BF16) for _ in range(2)]
        y2 = A.alloc([128, 512], F32)
        tt = A.alloc([128, 512], F32)
        sg = A.alloc([128, 512], F32)
        obs = [A.alloc([128, 512], BF16) for _ in range(2)]
        robs = [Res("s5o0"), Res("s5o1")]
        rt = Res("s5t")
        tch = TCH if need_ctx else TCH[:4]
        for j in range(2):
            for (t0, n) in tch:
                sl = slice(t0, t0 + n)
                yv = yacc[:, j, sl]
                em.op("dve", lambda e, j=j, sl=sl, yv=yv: e.scalar_tensor_tensor(out=yv, in0=ub[j][:, sl], scalar=self.s5cols[:, j:j + 1], in1=yv, op0=ALU.mult, op1=ALU.add), r=[ru, self.r_s5w], w=[ry])
                em.op("act", lambda e, yv=yv, n=n: e.activation(out=y2[:, 0:n], in_=yv, func=AF.Square), r=[ry], w=[rt])
                em.op("dve", lambda e, n=n: e.tensor_scalar(out=tt[:, 0:n], in0=y2[:, 0:n], scalar1=0.044715, scalar2=1.0, op0=ALU.mult, op1=ALU.add), r=[rt], w=[rt])
                em.op("dve", lambda e, yv=yv, n=n: e.tensor_tensor(out=tt[:, 0:n], in0=tt[:, 0:n], in1=yv, op=ALU.mult), r=[rt, ry], w=[rt])
                em.op("act", lambda e, n=n: e.activation(out=sg[:, 0:n], in_=tt[:, 0:n], func=AF.Sigmoid, scale=1.5957691216057308), r=[rt], w=[rt])
                em.op("dve", lambda e, j=j, sl=sl, yv=yv, n=n: e.tensor_tensor(out=gT[j][:, sl], in0=sg[:, 0:n], in1=yv, op=ALU.mult), r=[rt, ry], w=[rg])
        oi = 0
        for jo in range(2):
            for (t0, n) in tch:
                sl = slice(t0, t0 + n)
                ps, pr = self.bank("g")
                for j in range(2):
                    em.op("pe", lambda e, ps=ps, j=j, jo=jo, sl=sl, n=n: e.matmul(out=ps[:, 0:n], lhsT=self.wglu[:, j, jo * 128:(jo + 1) * 128], rhs=gT[j][:, sl], start=(j == 0), stop=(j == 1)), r=[rg, self.r_s5w], w=[pr])
                em.op("act", lambda e, ps=ps, jo=jo, n=n: e.activation(out=sg[:, 0:n], in_=ps[:, 0:n], func=AF.Sigmoid, bias=self.s5cols[:, 2 + jo:3 + jo]), r=[pr, self.r_s5w], w=[rt])
                ob, rob = obs[oi % 2], robs[oi % 2]
                oi += 1
                em.op("dve", lambda e, ob=ob, jo=jo, sl=sl, n=n: e.tensor_tensor(out=ob[:, 0:n], in0=sg[:, 0:n], in1=gT[jo][:, sl], op=ALU.mult), r=[rt, rg], w=[rob])
                em.dma("pool", lambda e, ob=ob, jo=jo, t0=t0, n=n: e.dma_start(out=mix[b, 512 + jo * 128:512 + (jo + 1) * 128, t0:t0 + n], in_=ob[:, 0:n]), r=[rob], w=[self.R["mix"]])
        em.barrier()
        A.pop()


    def moe_layer_state(self, l, need_ctx):
        em, A, BL = self.em, self.A, self.BL
        d = self.din
        self.TPB = 18 if need_ctx else 16
        self.NT = NT = BL * self.TPB
        self.NBLK = (NT * 128 * 2 + 32 * 127 + 127) // 128
        NTm = BL * 18
        self.Oall = A.alloc([128, NTm * 2, 32], BF16)
        self.pos = A.alloc([128, NTm * 2], F32)
        self.gates = A.alloc([128, NTm * 2], F32)
        self.carry = A.alloc([128, 32], F32)
        self.r_moe = Res("moe_state")
        em.op("dve", lambda e: e.memset(self.carry, 0.0), w=[self.r_moe])
        self.wout = A.alloc([128, 8, D], BF16)
        self.wr = A.alloc([128, 8, 36], F32)
        self.rbias = A.alloc([128, 36], F32)
        self.r_wout = Res("wout")
        for k in range(8):
            em.dma("pool", lambda e, k=k: e.dma_start(out=self.wout[:, k, :], in_=d["w_out"][l, k * 128:(k + 1) * 128, :]), r=[], w=[self.r_wout])
            em.dma("sp", lambda e, k=k: e.dma_start(out=self.wr[:, k, 0:4], in_=d["moe_w_group"][l, k * 128:(k + 1) * 128, :]), r=[], w=[self.r_wout])
            em.dma("sp", lambda e, k=k: e.dma_start(out=self.wr[:, k, 4:36], in_=d["moe_w_expert"][l, k * 128:(k + 1) * 128, :]), r=[], w=[self.r_wout])
        em.dma("sp", lambda e: e.dma_start(out=self.rbias[:, 0:4], in_=d["moe_b_group"][l].partition_broadcast(128)), r=[], w=[self.r_wout])
        em.dma("sp", lambda e: e.dma_start(out=self.rbias[:, 4:36], in_=d["moe_b_expert"][l].partition_broadcast(128)), r=[], w=[self.r_wout])

    def stage_outproj(self, l, b, need_ctx, xsrc, xres):
        em, A, BL = self.em, self.A, self.BL
        mix, xs, hall = self.dsc["mix"], self.dsc["xs"], self.dsc["hall"]
        A.push()
        mt = [A.alloc([128, 8, 512], BF16) for _ in range(2)]
        xt = [A.alloc([128, 8, 512], F32) for _ in range(2)]
        rmt = [Res("mt0"), Res("mt1")]
        rxt = [Res("oxt0"), Res("oxt1")]
        xn = A.alloc([128, 8, 512], F32)
        rxn = Res("xn")
        sq = A.alloc([128, 8, 512], BF16)
        tmp = A.alloc([128, 8, 512], F32)
        rstd = A.alloc([128, 512], F32)
        rtmp = Res("otmp")
        hb = A.alloc([128, 8, 512], BF16)
        hf = A.alloc([128, 8, 512], F32)
        rh = Res("oh")
        htok = [A.alloc([128, D], BF16) for _ in range(2)]
        rhtok = [Res("htok0"), Res("htok1")]
        Lg = A.alloc([128, 36], F32)
        sm = A.alloc([128, 64], F32)
        oh1 = A.alloc([128, 8], F32)
        oh2 = A.alloc([128, 8], F32)
        es = A.alloc([128, 8], F32)
        e2 = A.alloc([128, 8], F32)
        goh = A.alloc([128, 4], F32)
        t4 = A.alloc([128, 4], F32)
        t32 = A.alloc([128, 32], F32)
        rr = Res("route")
        xv = xsrc[b].rearrange("(k p) t -> p k t", p=128)
        xo = xs[b].rearrange("(k p) t -> p k t", p=128)
        mv = mix[b].rearrange("(k p) t -> p k t", p=128)
        tch = TCH if need_ctx else TCH[:4]
        hi = 0
        for ci, (t0, n) in enumerate(tch):
            m_, rm_, x_, rx_ = mt[ci % 2], rmt[ci % 2], xt[ci % 2], rxt[ci % 2]
            bcol = b if t0 < TL else BL
            em.dma("sp", lambda e, m_=m_, t0=t0, n=n: e.dma_start(out=m_[:, :, 0:n], in_=mv[:, :, t0:t0 + n]), r=[self.R["mix"]], w=[rm_])
            em.dma("sp", lambda e, x_=x_, t0=t0, n=n: e.dma_start(out=x_[:, :, 0:n], in_=xv[:, :, t0:t0 + n]), r=[xres], w=[rx_])
            for dj in range(8):
                ps, pr = self.bank("g")
                for k in range(8):
                    em.op("pe", lambda e, ps=ps, k=k, dj=dj, m_=m_, n=n: e.matmul(out=ps[:, 0:n], lhsT=self.wout[:, k, dj * 128:(dj + 1) * 128], rhs=m_[:, k, 0:n], start=(k == 0), stop=(k == 7)), r=[self.r_wout, rm_], w=[pr])
                em.op("dve", lambda e, ps=ps, dj=dj, x_=x_, n=n, bcol=bcol: e.scalar_tensor_tensor(out=xn[:, dj, 0:n], in0=ps[:, 0:n], scalar=self.mod[:, l, 16 + dj, bcol:bcol + 1], in1=x_[:, dj, 0:n], op0=ALU.mult, op1=ALU.add), r=[pr, rx_, self.r_mod], w=[rxn])
            em.dma("pool", lambda e, t0=t0, n=n: e.dma_start(out=xo[:, :, t0:t0 + n], in_=xn[:, :, 0:n]), r=[rxn], w=[self.R["xs"]])
            self.norm_mod_chunk(xn, n, hb, self.gsc2[:, l], self.mod[:, l, 24:32], bcol, rxn, rh, tmp, rstd, sq, rtmp, f32_out=hf)
            for tt in range(n // 128):
                ti = b * self.TPB + (t0 // 128 + tt)
                cs_ = slice(tt * 128, (tt + 1) * 128)
                pt, prt = self.bank("m")
                ptb = pt.bitcast(BF16)
                for k in range(8):
                    em.op("pe", lambda e, ptb=ptb, k=k, cs_=cs_: e.transpose(out=ptb[:, k * 128:(k + 1) * 128], in_=hb[:, k, cs_], identity=self.ident_bf), r=[rh, self.r_const], w=[prt])
                ht, rht = htok[hi % 2], rhtok[hi % 2]
                hi += 1
                em.op("act", lambda e, ptb=ptb, ht=ht: e.activation(out=ht, in_=ptb, func=AF.Copy), r=[prt], w=[rht])
                em.dma("pool", lambda e, ht=ht, ti=ti: e.dma_start(out=hall[ti * 128:(ti + 1) * 128, :], in_=ht), r=[rht], w=[self.R["hall"]])
                pl, prl = self.bank("m")
                for k in range(8):
                    em.op("pe", lambda e, pl=pl, k=k, cs_=cs_: e.matmul(out=pl[:, 0:36], lhsT=hf[:, k, cs_], rhs=self.wr[:, k, :], start=(k == 0), stop=(k == 7)), r=[rh, self.r_wout], w=[prl])
                V = lambda fn, r=(), w=(): em.op("dve", fn, r=[rr] + list(r), w=[rr] + list(w))
                V(lambda e, pl=pl: e.tensor_tensor(out=Lg, in0=pl[:, 0:36], in1=self.rbias, op=ALU.add), r=[prl, self.r_wout])
                gm, ngm, gsum, ggate, m1, m2, dm, ex, w1, g1c, g2c = (sm[:, i:i + 1] for i in range(11))
                V(lambda e: e.reduce_max(out=gm, in_=Lg[:, 0:4], axis=AX.X))
                V(lambda e: e.tensor_scalar(out=goh, in0=Lg[:, 0:4], scalar1=gm, scalar2=None, op0=ALU.is_ge))
                V(lambda e: e.tensor_scalar(out=ngm, in0=gm, scalar1=-1.0, scalar2=None, op0=ALU.mult))
                em.op("act", lambda e: e.activation(out=t4, in_=Lg[:, 0:4], func=AF.Exp, bias=ngm, scale=1.0, accum_out=gsum), r=[rr], w=[rr])
                V(lambda e: e.reciprocal(out=ggate, in_=gsum))
                V(lambda e: e.tensor_scalar(out=es, in0=Lg[:, 4:12], scalar1=goh[:, 0:1], scalar2=None, op0=ALU.mult))
                for g in range(1, 4):
                    V(lambda e, g=g: e.scalar_tensor_tensor(out=es, in0=Lg[:, 4 + 8 * g:12 + 8 * g], scalar=goh[:, g:g + 1], in1=es, op0=ALU.mult, op1=ALU.add))
                V(lambda e: e.reduce_max(out=m1, in_=es, axis=AX.X))
                V(lambda e: e.tensor_scalar(out=oh1, in0=es, scalar1=m1, scalar2=None, op0=ALU.is_ge))
                V(lambda e: e.scalar_tensor_tensor(out=e2, in0=oh1, scalar=-1.0e30, in1=es, op0=ALU.mult, op1=ALU.add))
                V(lambda e: e.reduce_max(out=m2, in_=e2, axis=AX.X))
                V(lambda e: e.tensor_scalar(out=oh2, in0=e2, scalar1=m2, scalar2=None, op0=ALU.is_ge))
                V(lambda e: e.tensor_tensor(out=dm, in0=m2, in1=m1, op=ALU.subtract))
                em.op("act", lambda e: e.activation(out=ex, in_=dm, func=AF.Exp), r=[rr], w=[rr])
                V(lambda e: e.tensor_scalar(out=w1, in0=ex, scalar1=1.0, scalar2=None, op0=ALU.add))
                V(lambda e: e.reciprocal(out=w1, in_=w1))
                V(lambda e, ti=ti: e.tensor_tensor(out=self.gates[:, 2 * ti:2 * ti + 1], in0=w1, in1=ggate, op=ALU.mult), w=[self.r_moe])
                V(lambda e, ti=ti: e.scalar_tensor_tensor(out=self.gates[:, 2 * ti + 1:2 * ti + 2], in0=w1, scalar=ex, in1=ggate, op0=ALU.mult, op1=ALU.mult), w=[self.r_moe])
                for kk, oh in enumerate((oh1, oh2)):
                    slot = 2 * ti + kk
                    for g in range(4):
                        V(lambda e, g=g, oh=oh, slot=slot: e.tensor_scalar(out=self.Oall[:, slot, 8 * g:8 * g + 8], in0=oh, scalar1=goh[:, g:g + 1], scalar2=None, op0=ALU.mult), w=[self.r_moe])
                    pk, prk = self.bank("m")
                    em.op("pe", lambda e, pk=pk, slot=slot: e.matmul(out=pk[:, 0:32], lhsT=self.utri_bf, rhs=self.Oall[:, slot, :], start=True, stop=True), r=[self.r_moe, self.r_const], w=[prk])
                    em.op("pe", lambda e, pk=pk, slot=slot: e.matmul(out=pk[:, 32:64], lhsT=self.ones_bf, rhs=self.Oall[:, slot, :], start=True, stop=True), r=[self.r_moe, self.r_const], w=[prk])
                    V(lambda e, pk=pk: e.tensor_tensor(out=t32, in0=pk[:, 0:32], in1=self.carry, op=ALU.add), r=[prk, self.r_moe])
                    V(lambda e, slot=slot: e.tensor_tensor(out=t32, in0=t32, in1=self.Oall[:, slot, :], op=ALU.mult), r=[self.r_moe])
                    V(lambda e, slot=slot: e.reduce_sum(out=self.pos[:, slot:slot + 1], in_=t32, axis=AX.X), w=[self.r_moe])
                    V(lambda e, pk=pk: e.tensor_tensor(out=self.carry, in0=pk[:, 32:64], in1=self.carry, op=ALU.add), r=[prk], w=[self.r_moe])
        em.barrier()
        A.pop()

    def stage_moe(self, l, need_ctx):
        em, A, BL = self.em, self.A, self.BL
        d = self.din
        NT, NBLK, TPB = self.NT, self.NBLK, self.TPB
        hall, hbuf, ybuf, xs = self.dsc["hall"], self.dsc["hbuf"], self.dsc["ybuf"], self.dsc["xs"]
        A.push()
        rr = Res("disp")
        V = lambda fn, r=(), w=(): em.op("dve", fn, r=[rr, self.r_moe, self.r_const] + list(r), w=[rr] + list(w))
        cmp_ = A.alloc([128, 32, 192], F32)
        nb = A.alloc([128, 32], F32)
        padded = A.alloc([128, 32], F32)
        pend = A.alloc([128, 32], F32)
        pstart = A.alloc([128, 32], F32)
        ones32 = A.alloc([128, 32], F32)
        thr = self.cst2[:, 128:320]
        V(lambda e: e.memset(ones32, 1.0))
        V(lambda e: e.tensor_tensor(out=cmp_, in0=self.carry.unsqueeze(2).to_broadcast([128, 32, 192]), in1=thr.unsqueeze(1).to_broadcast([128, 32, 192]), op=ALU.is_gt))
        V(lambda e: e.reduce_sum(out=nb, in_=cmp_, axis=AX.X))
        V(lambda e: e.tensor_scalar(out=padded, in0=nb, scalar1=128.0, scalar2=None, op0=ALU.mult))
        V(lambda e: e.tensor_tensor_scan(out=pend, data0=ones32, data1=padded, initial=0.0, op0=ALU.mult, op1=ALU.add))
        V(lambda e: e.tensor_tensor(out=pstart, in0=pend, in1=padded, op=ALU.subtract))
        dtmp = A.alloc([128, NT * 2, 32], F32)
        destf = A.alloc([128, NT * 2], F32)
        desti = A.alloc([128, NT * 2], I32)
        V(lambda e: e.tensor_tensor(out=dtmp, in0=self.Oall[:, 0:NT * 2, :], in1=pstart.unsqueeze(1).to_broadcast([128, NT * 2, 32]), op=ALU.mult))
        V(lambda e: e.reduce_sum(out=destf, in_=dtmp, axis=AX.X))
        V(lambda e: e.tensor_tensor(out=destf, in0=destf, in1=self.pos[:, 0:NT * 2], op=ALU.add))
        V(lambda e: e.tensor_copy(out=desti, in_=destf))
        bcmp = A.alloc([128, 192, 32], F32)
        be = A.alloc([128, 192], F32)
        idx1f = A.alloc([128, 192, 8], F32)
        idx2f = A.alloc([128, 192, 4], F32)
        idx1 = A.alloc([128, 192, 8], I32)
        idx2 = A.alloc([128, 192, 4], I32)
        pcol = self.cst2[:, 320:321]
        V(lambda e: e.tensor_tensor(out=bcmp, in0=thr.unsqueeze(2).to_broadcast([128, 192, 32]), in1=pend.unsqueeze(1).to_broadcast([128, 192, 32]), op=ALU.is_ge))
        V(lambda e: e.reduce_sum(out=be, in_=bcmp, axis=AX.X))
        V(lambda e: e.tensor_scalar(out=be, in0=be, scalar1=31.0, scalar2=None, op0=ALU.min))
        for kc in range(8):
            V(lambda e, kc=kc: e.tensor_scalar(out=idx1f[:, :, kc], in0=be, scalar1=1024.0, scalar2=float(kc * 128 + l * NE * D), op0=ALU.mult, op1=ALU.add))
        for fc in range(4):
            V(lambda e, fc=fc: e.tensor_scalar(out=idx2f[:, :, fc], in0=be, scalar1=512.0, scalar2=float(fc * 128 + l * NE * FF), op0=ALU.mult, op1=ALU.add))
        V(lambda e: e.tensor_scalar(out=idx1f, in0=idx1f, scalar1=pcol, scalar2=None, op0=ALU.add))
        V(lambda e: e.tensor_scalar(out=idx2f, in0=idx2f, scalar1=pcol, scalar2=None, op0=ALU.add))
        V(lambda e: e.tensor_copy(out=idx1, in_=idx1f))
        V(lambda e: e.tensor_copy(out=idx2, in_=idx2f))
        if "dbg_moe" in self.dbg:
            dbg = self.scratch("dbg_moe", [128, NT * 2 * 3 + 192 + 64], F32)
            for o_, t_, n_ in ((0, destf, NT * 2), (NT * 2, self.pos[:, 0:NT * 2], NT * 2), (NT * 4, self.gates[:, 0:NT * 2], NT * 2), (NT * 6, be, 192), (NT * 6 + 192, self.carry, 32), (NT * 6 + 224, pend, 32)):
                em.dma("sp", lambda e, o_=o_, t_=t_, n_=n_: e.dma_start(out=dbg[:, o_:o_ + n_], in_=t_), r=[rr, self.r_moe], w=[self.R["dbg_moe"]])
        A.push()
        hb2 = [A.alloc([128, D], BF16) for _ in range(3)]
        rhb2 = [Res("hb2_%d" % i) for i in range(3)]
        for ti in range(NT):
            t_, rt_ = hb2[ti % 3], rhb2[ti % 3]
            em.dma("sp", lambda e, t_=t_, ti=ti: e.dma_start(out=t_, in_=hall[ti * 128:(ti + 1) * 128, :]), r=[self.R["hall"]], w=[rt_])
            for kk in range(2):
                slot = 2 * ti + kk
                em.dma("pool", lambda e, t_=t_, slot=slot: e.indirect_dma_start(
                    out=hbuf[:, :], out_offset=bass.IndirectOffsetOnAxis(ap=desti[:, slot:slot + 1], axis=0),
                    in_=t_, in_offset=None), r=[rt_, rr], w=[self.R["hbuf"]])
        em.barrier()
        A.pop()
        A.push()
        w1v = d["moe_w1"].rearrange("l e d f -> (l e d) f")
        w3v = d["moe_w3"].rearrange("l e d f -> (l e d) f")
        w2v = d["moe_w2"].rearrange("l e f d -> (l e f) d")
        NB_ = 2
        w1g = [A.alloc([128, 8, FF], BF16) for _ in range(NB_)]
        w3g = [A.alloc([128, 8, FF], BF16) for _ in range(NB_)]
        w2g = [A.alloc([128, 4, D], BF16) for _ in range(NB_)]
        rw = [Res("wg%d" % i) for i in range(NB_)]
        hbk = [A.alloc([128, D], BF16) for _ in range(2)]
        rhbk = [Res("hbk0"), Res("hbk1")]
        hT = A.alloc([128, 8, 128], BF16)
        rhT = Res("hT")
        sl_ = A.alloc([128, FF], F32)
        actb = A.alloc([128, FF], BF16)
        actT = A.alloc([128, 4, 128], BF16)
        ract = Res("act")
        yb = [A.alloc([128, D], F32) for _ in range(2)]
        ryb = [Res("yb0"), Res("yb1")]
        for blk in range(NBLK):
            i = blk % NB_
            for kc in range(8):
                em.dma("pool", lambda e, i=i, kc=kc, blk=blk: e.indirect_dma_start(
                    out=w1g[i][:, kc, :], out_offset=None, in_=w1v, in_offset=bass.IndirectOffsetOnAxis(ap=idx1[:, blk, kc:kc + 1], axis=0)), r=[rr], w=[rw[i]])
                em.dma("pool", lambda e, i=i, kc=kc, blk=blk: e.indirect_dma_start(
                    out=w3g[i][:, kc, :], out_offset=None, in_=w3v, in_offset=bass.IndirectOffsetOnAxis(ap=idx1[:, blk, kc:kc + 1], axis=0)), r=[rr], w=[rw[i]])
            for fc in range(4):
                em.dma("pool", lambda e, i=i, fc=fc, blk=blk: e.indirect_dma_start(
                    out=w2g[i][:, fc, :], out_offset=None, in_=w2v, in_offset=bass.IndirectOffsetOnAxis(ap=idx2[:, blk, fc:fc + 1], axis=0)), r=[rr], w=[rw[i]])
            hk, rhk = hbk[blk % 2], rhbk[blk % 2]
            em.dma("sp", lambda e, hk=hk, blk=blk: e.dma_start(out=hk, in_=hbuf[blk * 128:(blk + 1) * 128, :]), r=[self.R["hbuf"]], w=[rhk])
            pt, prt = self.bank("m")
            ptb = pt.bitcast(BF16)
            for k in range(8):
                em.op("pe", lambda e, ptb=ptb, k=k, hk=hk: e.transpose(out=ptb[:, k * 128:(k + 1) * 128], in_=hk[:, k * 128:(k + 1) * 128], identity=self.ident_bf), r=[rhk, self.r_const], w=[prt])
            em.op("act", lambda e, ptb=ptb: e.activation(out=hT.rearrange("p k c -> p (k c)"), in_=ptb, func=AF.Copy), r=[prt], w=[rhT])
            p1, pr1 = self.bank("g")
            p3, pr3 = self.bank("g")
            for k in range(8):
                em.op("pe", lambda e, p1=p1, k=k, i=i: e.matmul(out=p1[:, :], lhsT=hT[:, k, :], rhs=w1g[i][:, k, :], start=(k == 0), stop=(k == 7)), r=[rhT, rw[i]], w=[pr1])
            for k in range(8):
                em.op("pe", lambda e, p3=p3, k=k, i=i: e.matmul(out=p3[:, :], lhsT=hT[:, k, :], rhs=w3g[i][:, k, :], start=(k == 0), stop=(k == 7)), r=[rhT, rw[i]], w=[pr3])
            em.op("act", lambda e, p1=p1: e.activation(out=sl_, in_=p1[:, :], func=AF.Silu), r=[pr1], w=[ract])
            em.op("dve", lambda e, p3=p3: e.tensor_tensor(out=actb, in0=p3[:, :], in1=sl_, op=ALU.mult), r=[pr3, ract], w=[ract])
            pa, pra = self.bank("m")
            pab = pa.bitcast(BF16)
            for fc in range(4):
                em.op("pe", lambda e, pab=pab, fc=fc: e.transpose(out=pab[:, fc * 128:(fc + 1) * 128], in_=actb[:, fc * 128:(fc + 1) * 128], identity=self.ident_bf), r=[ract, self.r_const], w=[pra])
            em.op("act", lambda e, pab=pab: e.activation(out=actT.rearrange("p k c -> p (k c)"), in_=pab[:, 0:512], func=AF.Copy), r=[pra], w=[ract])
            y_, ry_ = yb[blk % 2], ryb[blk % 2]
            for half in range(2):
                py, pry = self.bank("g")
                for fc in range(4):
                    em.op("pe", lambda e, py=py, fc=fc, i=i, half=half: e.matmul(out=py[:, :], lhsT=actT[:, fc, :], rhs=w2g[i][:, fc, half * 512:(half + 1) * 512], start=(fc == 0), stop=(fc == 3)), r=[ract, rw[i]], w=[pry])
                if half == 0:
                    em.op("act", lambda e, py=py, y_=y_: e.activation(out=y_[:, 0:512], in_=py[:, :], func=AF.Copy), r=[pry], w=[ry_])
                else:
                    em.op("dve", lambda e, py=py, y_=y_: e.tensor_copy(out=y_[:, 512:1024], in_=py[:, :]), r=[pry], w=[ry_])
            em.dma("sp", lambda e, y_=y_, blk=blk: e.dma_start(out=ybuf[blk * 128:(blk + 1) * 128, :], in_=y_), r=[ry_], w=[self.R["ybuf"]])
        em.barrier()
        A.pop()
        A.push()
        y1 = [A.alloc([128, D], F32) for _ in range(2)]
        y2 = [A.alloc([128, D], F32) for _ in range(2)]
        ry1 = [Res("y1_0"), Res("y1_1")]
        ry2 = [Res("y2_0"), Res("y2_1")]
        ym = A.alloc([128, D], F32)
        rym = Res("ym")
        xc_ = [A.alloc([128, 8, 128], F32) for _ in range(2)]
        rxc = [Res("xc0"), Res("xc1")]
        xo_ = [A.alloc([128, 8, 128], F32) for _ in range(2)]
        rxo = [Res("xo0"), Res("xo1")]
        for ti in range(NT):
            b, tloc = ti // TPB, ti % TPB
            t0 = tloc * 128
            bcol = b if t0 < TL else BL
            i = ti % 2
            for kk, (yy, ryy) in enumerate(((y1[i], ry1[i]), (y2[i], ry2[i]))):
                slot = 2 * ti + kk
                em.dma("pool", lambda e, yy=yy, slot=slot: e.indirect_dma_start(
                    out=yy, out_offset=None, in_=ybuf[:, :], in_offset=bass.IndirectOffsetOnAxis(ap=desti[:, slot:slot + 1], axis=0)),
                    r=[self.R["ybuf"], rr], w=[ryy])
            xv = xs[b].rearrange("(k p) t -> p k t", p=128)
            em.dma("sp", lambda e, i=i, xv=xv, t0=t0: e.dma_start(out=xc_[i], in_=xv[:, :, t0:t0 + 128]), r=[self.R["xs"]], w=[rxc[i]])
            em.op("dve", lambda e, i=i, ti=ti: e.tensor_scalar(out=ym, in0=y1[i], scalar1=self.gates[:, 2 * ti:2 * ti + 1], scalar2=None, op0=ALU.mult), r=[ry1[i], self.r_moe], w=[rym])
            em.op("dve", lambda e, i=i, ti=ti: e.scalar_tensor_tensor(out=ym, in0=y2[i], scalar=self.gates[:, 2 * ti + 1:2 * ti + 2], in1=ym, op0=ALU.mult, op1=ALU.add), r=[ry2[i], self.r_moe], w=[rym])
            for half in range(2):
                pt, prt = self.bank("g")
                for kq in range(4):
                    k = half * 4 + kq
                    em.op("pe", lambda e, pt=pt, k=k, kq=kq: e.transpose(out=pt[:, kq * 128:(kq + 1) * 128], in_=ym[:, k * 128:(k + 1) * 128], identity=self.cst[:, 0:128]), r=[rym, self.r_const], w=[prt])
                for kq in range(4):
                    k = half * 4 + kq
                    em.op("dve", lambda e, pt=pt, k=k, kq=kq, i=i, bcol=bcol: e.scalar_tensor_tensor(out=xo_[i][:, k, :], in0=pt[:, kq * 128:(kq + 1) * 128], scalar=self.mod[:, l, 40 + k, bcol:bcol + 1], in1=xc_[i][:, k, :], op0=ALU.mult, op1=ALU.add), r=[prt, rxc[i], self.r_mod], w=[rxo[i]])
            em.dma("sp", lambda e, i=i, xv=xv, t0=t0: e.dma_start(out=xv[:, :, t0:t0 + 128], in_=xo_[i]), r=[rxo[i]], w=[self.R["xs"]])
        em.barrier()
        A.pop()
        A.pop()

    def stage_final(self, b):
        em, A = self.em, self.A
        d = self.din
        xs, outT = self.dsc["xs"], self.dsc["outT"]
        A.push()
        gf = A.alloc([128, 8], F32)
        rgf = Res("gf")
        em.dma("sp", lambda e: e.dma_start(out=gf, in_=d["g_final"].rearrange("(k p) -> p k", p=128), allow_slow_non_contiguous=True), r=[], w=[rgf])
        xt = [A.alloc([128, 8, 512], F32) for _ in range(2)]
        rx = [Res("fx0"), Res("fx1")]
        ot = [A.alloc([128, 8, 512], F32) for _ in range(2)]
        ro = [Res("fo0"), Res("fo1")]
        sq = A.alloc([128, 8, 512], BF16)
        rstd = A.alloc([128, 512], F32)
        rt = Res("ft")
        xv = xs[b].rearrange("(k p) t -> p k t", p=128)
        ov = outT[b].rearrange("(k p) t -> p k t", p=128)
        for ci, (t0, n) in enumerate(TCH[:4]):
            i = ci % 2
            em.dma("sp", lambda e, i=i, t0=t0: e.dma_start(out=xt[i], in_=xv[:, :, t0:t0 + 512]), r=[self.R["xs"]], w=[rx[i]])
            em.op("act", lambda e, i=i: e.activation(out=sq, in_=xt[i], func=AF.Square), r=[rx[i]], w=[rt])
            ps, pr = self.bank("g")
            for k in range(8):
                em.op("pe", lambda e, ps=ps, k=k: e.matmul(out=ps[:, :], lhsT=self.ones_bf, rhs=sq[:, k, :], start=(k == 0), stop=(k == 7)), r=[rt, self.r_const], w=[pr])
            em.op("act", lambda e, ps=ps: e.activation(out=rstd, in_=ps[:, :], func=AF.Sqrt, bias=self.eps_col, scale=1.0 / D), r=[pr, self.r_const], w=[rt])
            em.op("dve", lambda e: e.reciprocal(out=rstd, in_=rstd), r=[rt], w=[rt])
            for k in range(8):
                em.op("dve", lambda e, i=i, k=k: e.scalar_tensor_tensor(out=ot[i][:, k, :], in0=xt[i][:, k, :], scalar=gf[:, k:k + 1], in1=rstd, op0=ALU.mult, op1=ALU.mult), r=[rx[i], rt, rgf], w=[ro[i]])
            em.dma("sp", lambda e, i=i, t0=t0: e.dma_start(out=ov[:, :, t0:t0 + 512], in_=ot[i]), r=[ro[i]], w=[self.R["outT"]], is_out=True)
        em.barrier()
        A.pop()


def rope_perm(R):
    h = R // 2
    q = h // 2
    perm = np.zeros(R, np.int64)
    sign = np.zeros(R, np.float32)
    for j in range(R):
        jj = j % h
        if jj < q:
            perm[j] = j + q
            sign[j] = -1.0
        else:
            perm[j] = j - q
            sign[j] = 1.0
    return perm, sign


def rope_tables(R):
    t = np.arange(TL)
    row = (t // 64).astype(np.float32)
    col = (t % 64).astype(np.float32)
    half = R // 2
    inv = (1.0 / (np.float32(10000.0) ** (np.arange(0, half, 2, dtype=np.float32) / np.float32(half)))).astype(np.float32)
    ar = row[:, None] * inv
    ac = col[:, None] * inv
    ang = np.concatenate([ar, ar, ac, ac], axis=-1).astype(np.float32)
    _, sign = rope_perm(R)
    tab = np.zeros((2, R, T), np.float32)
    tab[0, :, :TL] = np.cos(ang).T
    tab[0, :, TL:] = 1.0
    tab[1, :, :TL] = (np.sin(ang) * sign[None, :]).T
    return tab


NA_PAIRS = Builder.NA_PAIRS


def na_toeplitz(rpb):
    L = rpb.shape[0]
    kc = np.arange(64)[:, None]
    qc = np.arange(64)[None, :]
    dc = np.clip(kc - qc + 15, 0, 30)
    out = np.zeros((L, 4, 23, 64, 64), np.float32)
    for j in range(23):
        dr = 18 - j
        if 0 <= dr <= 14:
            out[:, :, j] = rpb[:, :, dr][:, :, dc]
    return out


def na_mask_const():
    m = np.full((28, 128, 512), NEG, np.float32)
    kc = np.arange(64)[:, None]
    qc = np.arange(64)[None, :]
    qs = np.clip(qc - 8, 0, 48)
    colv = (kc >= qs) & (kc < qs + 16)
    for pi, (qi, kt) in enumerate(NA_PAIRS):
        for krl in range(2):
            for qrl in range(8):
                kr, qr = 2 * kt + krl, 8 * qi + qrl
                rs = min(max(qr - 4, 0), 24)
                if rs <= kr < rs + 8:
                    blk = m[pi, krl * 64:(krl + 1) * 64, qrl * 64:(qrl + 1) * 64]
                    blk[colv] = 0.0
    return m


def ret_dist_const():
    BIG = np.float32(1.0e6)
    out = np.zeros((38, 2, 128, 512), np.float32)
    p = np.arange(128, dtype=np.float32)[:, None]
    j = np.arange(512, dtype=np.float32)[None, :]
    for tid in range(28):
        dl = np.float32(128 * tid - 1920) + j - p
        out[tid, 0] = np.where(dl >= 0, dl, BIG)
        out[tid, 1] = np.where(dl < 0, -dl, BIG)
    for qi in range(4):
        for c in range(2):
            m = 128 * c + p
            t = 512 * qi + j
            out[28 + qi * 2 + c, 0] = t + 256 - m
            out[28 + qi * 2 + c, 1] = 2048 - t + m
    for c in range(2):
        m = 128 * c + p
        t = j
        out[36 + c, 0] = np.where(m <= t, t - m, BIG)
        out[36 + c, 1] = np.where(m > t, m - t, BIG)
    return out


def const_block():
    c = np.zeros((128, 272), np.float32)
    c[:, 0:128] = np.eye(128, dtype=np.float32)
    for s_ in range(128):
        c[s_, 128 + (s_ + 64) % 128] = 1.0
    for p in range(128):
        c[p, 256 + p // 16] = 1.0
    c[:64, 264] = 1.0
    c[64:, 264] = -1.0
    c[:, 265] = -np.pi
    return c


def const_block2():
    c = np.zeros((128, 322), np.float32)
    for k in range(128):
        c[k, k + 1:128] = 1.0
    c[:, 128:320] = (128.0 * np.arange(192, dtype=np.float32))[None, :]
    c[:, 320] = np.arange(128, dtype=np.float32)
    return c


def host_layout(inputs, BL, cores):
    x, c, ctx, c_ctx = inputs["x"], inputs["c"], inputs["ctx"], inputs["c_ctx"]
    w_in = inputs["w_in"]
    pm, _ = rope_perm(32)
    pr_, _ = rope_perm(64)
    o = np.cumsum([0, 192, 128, 32, 256, 256, 256, 256, 256, 256, 256, 256])
    cols = []
    cols += list(range(o[0], o[3]))
    cols += [o[2] + int(p) for p in pm]
    cols += list(range(o[3], o[5]))
    cols += list(range(o[6], o[7]))
    cols += list(range(o[7], o[9]))
    cols += list(range(o[10], o[11]))
    for base in (o[7], o[8]):
        for hh in range(4):
            cols += [base + hh * 64 + int(p) for p in pr_]
    cols = np.asarray(cols)
    assert cols.shape[0] == NPROJ
    vcols = np.asarray(list(range(o[5], o[6])) + list(range(o[9], o[10])))
    shared = {
        "w_mod": inputs["w_mod"], "b_mod": inputs["b_mod"], "g_mix": inputs["g_mix"], "g_ffn": inputs["g_ffn"],
        "w_fm": np.ascontiguousarray(w_in[:, :, cols]), "w_v": np.ascontiguousarray(w_in[:, :, vcols]),
        "g_final": inputs["g_final"],
        "mla_g_cq": inputs["mla_g_cq"], "mla_g_ckv": inputs["mla_g_ckv"], "mla_w_uq": inputs["mla_w_uq"],
        "mla_w_ukv": inputs["mla_w_ukv"],
        "w_uq_rot": np.ascontiguousarray(
            inputs["mla_w_uq"].reshape(DEPTH, 192, 4, 96)[:, :, :, 64:96][:, :, :, pm]),
        "ropem": rope_tables(32), "roper": rope_tables(64),
        "na_tc": na_toeplitz(inputs["na_rpb"]), "na_mask": na_mask_const(),
        "ret_log_decay": inputs["ret_log_decay"], "retd": ret_dist_const(),
        "cst": const_block(), "cst2": const_block2(),
    }
    for k_ in ("w_out", "moe_w_group", "moe_b_group", "moe_w_expert", "moe_b_expert", "moe_w1", "moe_w3", "moe_w2"):
        shared[k_] = inputs[k_]
    for k_ in ("s5_a_re", "s5_a_im", "s5_log_dt", "s5_b_re", "s5_b_im", "s5_c_re", "s5_c_im", "s5_d", "s5_w_glu", "s5_b_glu"):
        shared[k_] = inputs[k_]
    maps = []
    for ci in range(cores):
        sl = slice(ci * BL, (ci + 1) * BL)
        xcat = np.concatenate([x[sl], ctx[sl]], axis=1)
        m = dict(shared)
        m["xin"] = np.ascontiguousarray(xcat.transpose(0, 2, 1))
        m["cc"] = np.ascontiguousarray(np.concatenate([c[sl], c_ctx[None]], axis=0))
        maps.append(m)
    return maps


_CACHE = {}


def kernel(**inputs):
    BL = inputs["x"].shape[0] // NCORES
    if "nc" not in _CACHE:
        _CACHE["nc"] = Builder(BL, DEPTH).build()
    nc = _CACHE["nc"]
    maps = host_layout(inputs, BL, NCORES)
    res = run_bass_kernel_spmd(nc, maps, core_ids=list(range(NCORES)))
    outs = [r["outT"] for r in res.results]
    full = np.concatenate(outs, axis=0).transpose(0, 2, 1)
    return np.ascontiguousarray(full).astype(np.float32)
```
